# Optimizing a Trainium2 kernel written in Bass

```python
import math
import jax, jax.numpy as jnp
from jax import lax
import numpy as np

D_MODEL = 2048
BATCH = 16
SEQ = 2048
DEPTH = 1

N_Q_HEADS = 16
N_KV_HEADS = 4
Q_GROUP = N_Q_HEADS // N_KV_HEADS
HEAD_DIM = 128
WINDOW = 128
BLOCK = 128
ATTN_WIDTH = N_Q_HEADS * HEAD_DIM
KV_WIDTH = N_KV_HEADS * HEAD_DIM

SSM_WIDTH = D_MODEL // 2
SSM_GROUP = 16
SSM_GROUPS = SSM_WIDTH // SSM_GROUP
SSM_STATE = 64
DT_MIN = 1e-3
DT_MAX = 1e-1

PEER_HEADS = 8
N_KEYS = 128
N_EXPERTS = N_KEYS * N_KEYS
PEER_QDIM = 256
PEER_HALF = PEER_QDIM // 2
PEER_TOPK = 16
PEER_TOKEN_BLOCK = 128

RMS_EPS = 1e-6
IN_WIDTH = ATTN_WIDTH + 2 * KV_WIDTH + SSM_WIDTH + 2 * D_MODEL
IN_SPLITS = (ATTN_WIDTH, ATTN_WIDTH + KV_WIDTH, ATTN_WIDTH + 2 * KV_WIDTH,
             ATTN_WIDTH + 2 * KV_WIDTH + SSM_WIDTH,
             ATTN_WIDTH + 2 * KV_WIDTH + SSM_WIDTH + D_MODEL)

kernel_name = "hybrid_swa_s5_peer_encoder"


def rms_norm(x, g):
    xf = x.astype(jnp.float32)
    y = xf * lax.rsqrt(jnp.mean(xf * xf, axis=-1, keepdims=True) + RMS_EPS)
    return (y * g.astype(jnp.float32)).astype(x.dtype)


def alibi_slopes(n_heads):
    return jnp.exp2(-8.0 * (jnp.arange(n_heads, dtype=jnp.float32) + 1.0) / n_heads)


def windowed_gqa(q, k, v, sink):
    b_, s_ = q.shape[0], q.shape[1]
    nb = s_ // BLOCK
    qb = q.reshape(b_, nb, BLOCK, N_KV_HEADS, Q_GROUP, HEAD_DIM).transpose(1, 0, 2, 3, 4, 5)
    pad = ((0, 0), (BLOCK, BLOCK), (0, 0), (0, 0))

    def band(t):
        tp = jnp.pad(t, pad).reshape(b_, nb + 2, BLOCK, N_KV_HEADS, HEAD_DIM)
        tw = jnp.concatenate([tp[:, :-2], tp[:, 1:-1], tp[:, 2:]], axis=2)
        return tw.transpose(1, 0, 2, 3, 4)

    kw, vw = band(k), band(v)
    slopes = alibi_slopes(N_Q_HEADS).reshape(N_KV_HEADS, Q_GROUP)[:, :, None, None]
    sink_f = sink.astype(jnp.float32).reshape(N_KV_HEADS, Q_GROUP)[:, :, None, None]
    scale = HEAD_DIM ** -0.5

    def block_attn(args):
        blk, qi, ki, vi = args
        qpos = blk * BLOCK + jnp.arange(BLOCK)
        kpos = (blk - 1) * BLOCK + jnp.arange(3 * BLOCK)
        dist = jnp.abs(qpos[:, None] - kpos[None, :])
        valid = (dist <= WINDOW) & (kpos >= 0) & (kpos < s_)
        s = jnp.einsum('bqkgd,bskd->bkgqs', qi.astype(jnp.float32), ki.astype(jnp.float32)) * scale
        s = s - slopes * dist.astype(jnp.float32)
        s = jnp.where(valid, s, -jnp.inf)
        m = jnp.maximum(jnp.max(s, axis=-1, keepdims=True), sink_f)
        p = jnp.exp(s - m)
        denom = jnp.sum(p, axis=-1, keepdims=True) + jnp.exp(sink_f - m)
        o = jnp.einsum('bkgqs,bskd->bqkgd', p / denom, vi.astype(jnp.float32))
        return o.astype(q.dtype)

    out = lax.map(block_attn, (jnp.arange(nb), qb, kw, vw))
    return out.transpose(1, 0, 2, 3, 4, 5).reshape(b_, s_, ATTN_WIDTH)


def _ssm_combine(left, right):
    a_l, b_l = left
    a_r, b_r = right
    return a_r * a_l, a_r * b_l + b_r


def s5_direction(u, a_re, a_im, log_step, b_re, b_im, c_re, c_im, reverse):
    s_ = u.shape[1]
    lam = lax.complex(a_re.astype(jnp.float32), a_im.astype(jnp.float32))
    step = jnp.exp(log_step.astype(jnp.float32))[:, None]
    lam_bar = jnp.exp(lam * step)
    b_mat = lax.complex(b_re.astype(jnp.float32), b_im.astype(jnp.float32))
    b_bar = ((lam_bar - 1.0) / lam)[:, :, None] * b_mat
    bu = jnp.einsum('bsgc,gnc->bsgn', u.astype(jnp.complex64), b_bar)
    a = jnp.broadcast_to(lam_bar[None, None], (1, s_, SSM_GROUPS, SSM_STATE))
    _, states = lax.associative_scan(_ssm_combine, (a, bu), reverse=reverse, axis=1)
    c_mat = lax.complex(c_re.astype(jnp.float32), c_im.astype(jnp.float32))
    return jnp.real(jnp.einsum('bsgn,gcn->bsgc', states, c_mat))


def peer(h, w_query, sub_keys_1, sub_keys_2, expert_down, expert_up):
    b_, s_, d_ = h.shape
    t_ = b_ * s_
    ht = h.reshape(t_, d_)
    q = (ht @ w_query).astype(jnp.float32).reshape(t_, PEER_HEADS, 2, PEER_HALF)
    s1 = jnp.einsum('thd,hkd->thk', q[:, :, 0], sub_keys_1.astype(jnp.float32))
    s2 = jnp.einsum('thd,hkd->thk', q[:, :, 1], sub_keys_2.astype(jnp.float32))
    v1, i1 = lax.top_k(s1, PEER_TOPK)
    v2, i2 = lax.top_k(s2, PEER_TOPK)
    cand = (v1[..., :, None] + v2[..., None, :]).reshape(t_, PEER_HEADS, PEER_TOPK * PEER_TOPK)
    cand_idx = (i1[..., :, None] * N_KEYS + i2[..., None, :]).reshape(t_, PEER_HEADS, PEER_TOPK * PEER_TOPK)
    top_s, pos = lax.top_k(cand, PEER_TOPK)
    idx = jnp.take_along_axis(cand_idx, pos, axis=-1)
    gate = jax.nn.softmax(top_s, axis=-1)
    nblk = t_ // PEER_TOKEN_BLOCK
    k_sel = PEER_HEADS * PEER_TOPK
    xs = (ht.reshape(nblk, PEER_TOKEN_BLOCK, d_),
          idx.reshape(nblk, PEER_TOKEN_BLOCK, k_sel),
          gate.reshape(nblk, PEER_TOKEN_BLOCK, k_sel))

    def token_block(args):
        hb, ib, gb = args
        u_sel = jnp.take(expert_down, ib, axis=0)
        act = jnp.einsum('tkd,td->tk', u_sel.astype(jnp.float32), hb.astype(jnp.float32))
        w = gb * jax.nn.gelu(act, approximate=False)
        v_sel = jnp.take(expert_up, ib, axis=0)
        return jnp.einsum('tk,tkd->td', w, v_sel.astype(jnp.float32))

    return lax.map(token_block, xs).reshape(b_, s_, d_)


def setup_inputs(seed: int = 0) -> dict:
    key = jax.random.key(seed)
    ks = jax.random.split(key, 24)
    L, D, f32 = DEPTH, D_MODEL, jnp.float32
    G, N, C = SSM_GROUPS, SSM_STATE, SSM_GROUP
    nrm = lambda k, shape, sc: jax.random.normal(k, shape, f32) * sc
    a_im_base = jnp.pi * jnp.arange(N, dtype=f32)
    return {
        "x": nrm(ks[0], (BATCH, SEQ, D), 1.0),
        "mix_norm_g": 1.0 + nrm(ks[1], (L, D), 0.02),
        "w_in": nrm(ks[2], (L, D, IN_WIDTH), D ** -0.5),
        "q_norm_g": 1.0 + nrm(ks[3], (L, HEAD_DIM), 0.02),
        "k_norm_g": 1.0 + nrm(ks[4], (L, HEAD_DIM), 0.02),
        "attn_sink": nrm(ks[5], (L, N_Q_HEADS), 0.5),
        "w_attn_o": nrm(ks[6], (L, ATTN_WIDTH, D), ATTN_WIDTH ** -0.5),
        "ssm_a_re": -0.5 + nrm(ks[7], (L, 2, G, N), 0.01),
        "ssm_a_im": a_im_base + nrm(ks[8], (L, 2, G, N), 0.01),
        "ssm_log_step": jax.random.uniform(ks[9], (L, 2, G), f32, math.log(DT_MIN), math.log(DT_MAX)),
        "ssm_b_re": nrm(ks[10], (L, 2, G, N, C), (2.0 * C) ** -0.5),
        "ssm_b_im": nrm(ks[11], (L, 2, G, N, C), (2.0 * C) ** -0.5),
        "ssm_c_re": nrm(ks[12], (L, 2, G, C, N), (2.0 * N) ** -0.5),
        "ssm_c_im": nrm(ks[13], (L, 2, G, C, N), (2.0 * N) ** -0.5),
        "ssm_d": nrm(ks[14], (L, SSM_WIDTH), 1.0),
        "glu_w_a": nrm(ks[15], (L, SSM_WIDTH, D), SSM_WIDTH ** -0.5),
        "glu_w_b": nrm(ks[16], (L, SSM_WIDTH, D), SSM_WIDTH ** -0.5),
        "w_out": nrm(ks[17], (L, D, D), D ** -0.5),
        "ffn_norm_g": 1.0 + nrm(ks[18], (L, D), 0.02),
        "peer_w_query": nrm(ks[19], (L, D, PEER_HEADS * PEER_QDIM), D ** -0.5),
        "peer_sub_keys_1": nrm(ks[20], (L, PEER_HEADS, N_KEYS, PEER_HALF), PEER_HALF ** -0.5),
        "peer_sub_keys_2": nrm(ks[21], (L, PEER_HEADS, N_KEYS, PEER_HALF), PEER_HALF ** -0.5),
        "peer_down": nrm(ks[22], (L, N_EXPERTS, D), D ** -0.5),
        "peer_up": nrm(ks[23], (L, N_EXPERTS, D), 0.25),
    }


def reference(x, mix_norm_g, w_in, q_norm_g, k_norm_g, attn_sink, w_attn_o,
              ssm_a_re, ssm_a_im, ssm_log_step, ssm_b_re, ssm_b_im, ssm_c_re, ssm_c_im, ssm_d,
              glu_w_a, glu_w_b, w_out, ffn_norm_g, peer_w_query, peer_sub_keys_1,
              peer_sub_keys_2, peer_down, peer_up):
    b_, s_ = x.shape[0], x.shape[1]
    for l in range(DEPTH):
        h = rms_norm(x, mix_norm_g[l])
        proj = h @ w_in[l]
        q, k, v, u, gate_a, gate_b = jnp.split(proj, IN_SPLITS, axis=-1)
        q = rms_norm(q.reshape(b_, s_, N_Q_HEADS, HEAD_DIM), q_norm_g[l])
        k = rms_norm(k.reshape(b_, s_, N_KV_HEADS, HEAD_DIM), k_norm_g[l])
        v = v.reshape(b_, s_, N_KV_HEADS, HEAD_DIM)
        y_a = windowed_gqa(q, k, v, attn_sink[l]) @ w_attn_o[l]

        ug = u.astype(jnp.float32).reshape(b_, s_, SSM_GROUPS, SSM_GROUP)
        y_fwd = s5_direction(ug, ssm_a_re[l, 0], ssm_a_im[l, 0], ssm_log_step[l, 0],
                             ssm_b_re[l, 0], ssm_b_im[l, 0], ssm_c_re[l, 0], ssm_c_im[l, 0], False)
        y_bwd = s5_direction(ug, ssm_a_re[l, 1], ssm_a_im[l, 1], ssm_log_step[l, 1],
                             ssm_b_re[l, 1], ssm_b_im[l, 1], ssm_c_re[l, 1], ssm_c_im[l, 1], True)
        y_ssm = (y_fwd + y_bwd).reshape(b_, s_, SSM_WIDTH) + ssm_d[l] * ug.reshape(b_, s_, SSM_WIDTH)
        y_act = jax.nn.gelu(y_ssm, approximate=False)
        y_b = (y_act @ glu_w_a[l]) * jax.nn.sigmoid(y_act @ glu_w_b[l])

        merged = jax.nn.sigmoid(gate_a) * y_a + jax.nn.sigmoid(gate_b) * y_b
        x = x + merged @ w_out[l]

        h2 = rms_norm(x, ffn_norm_g[l])
        x = x + peer(h2, peer_w_query[l], peer_sub_keys_1[l], peer_sub_keys_2[l],
                     peer_down[l], peer_up[l])
    return x
```

```python
import math
import numpy as np
import concourse.bass as bass
import concourse.mybir as mybir
from concourse.bass_utils import run_bass_kernel_spmd
from contextlib import ExitStack

F32 = mybir.dt.float32
BF16 = mybir.dt.bfloat16
I32 = mybir.dt.int32
ALU = mybir.AluOpType
AF = mybir.ActivationFunctionType
AX = mybir.AxisListType

SAME_ENGINE_SYNC = True
SEM_LIMIT = 30000


class Buf:
    __slots__ = ("t", "lw", "rd", "name")

    def __init__(self, t, name=""):
        self.t = t
        self.lw = None
        self.rd = {}
        self.name = name

    def __getitem__(self, k):
        return self.t[k]


class Op:
    __slots__ = ("eng", "fn", "deps", "is_dma", "key", "signal", "sem", "count", "phase")

    def __init__(self, eng, fn, deps, is_dma=False, key=None):
        self.eng = eng
        self.fn = fn
        self.deps = deps
        self.is_dma = is_dma
        self.key = key
        self.signal = is_dma
        self.sem = None
        self.count = 0


class Sched:
    ENG = ("pe", "act", "dve", "pool", "sp")

    def __init__(self, nc, stack):
        self.nc = nc
        self.stack = stack
        self.ops = []
        self.streams = {e: [] for e in self.ENG}
        self.key_last = {}
        self.nbuf = 0
        self.barrier_deps = {e: set() for e in self.ENG}
        self.phase = 0

    def sbuf(self, shape, dtype, name=None):
        self.nbuf += 1
        name = name or f"sb{self.nbuf}"
        t = self.stack.enter_context(self.nc.sbuf_tensor(name, list(shape), dtype))
        return Buf(t, name)

    def psum(self, shape, dtype, name=None):
        self.nbuf += 1
        name = name or f"ps{self.nbuf}"
        t = self.stack.enter_context(self.nc.psum_tensor(name, list(shape), dtype))
        return Buf(t, name)

    def dram(self, name, shape, dtype, kind="Internal"):
        t = self.nc.dram_tensor(name, list(shape), dtype, kind=kind)
        return Buf(t.ap(), name)

    def view(self, buf_ap, name=""):
        return Buf(buf_ap, name)

    def _mkdeps(self, eng, reads, writes, key_tag):
        deps = set()
        for b in reads:
            if b.lw is not None:
                deps.add(b.lw)
        for b in writes:
            if b.lw is not None:
                deps.add(b.lw)
            deps.update(b.rd.values())
        if self.barrier_deps[eng]:
            deps.update(self.barrier_deps[eng])
            self.barrier_deps[eng] = set()
        return deps

    def op(self, eng, fn, reads=(), writes=()):
        deps = self._mkdeps(eng, reads, writes, eng)
        oid = len(self.ops)
        self.ops.append(Op(eng, fn, deps))
        self.streams[eng].append(oid)
        for b in writes:
            b.lw = oid
            b.rd = {}
        ws = set(id(b) for b in writes)
        for b in reads:
            if id(b) not in ws:
                b.rd[eng] = oid
        return oid

    def dma(self, out_ap, in_ap, reads, writes, key, queue="sp", **kw):
        deps = self._mkdeps(queue, reads, writes, None)
        kid = id(key)
        if kid in self.key_last:
            deps.add(self.key_last[kid])
        oid = len(self.ops)

        def fn(e, out_ap=out_ap, in_ap=in_ap, kw=kw):
            return e.dma_start(out=out_ap, in_=in_ap, **kw)

        self.ops.append(Op(queue, fn, deps, True, key))
        self.ops[-1].phase = self.phase
        self.streams[queue].append(oid)
        self.key_last[kid] = oid
        for b in writes:
            b.lw = oid
            b.rd = {}
        ws = set(id(b) for b in writes)
        for b in reads:
            if id(b) not in ws:
                b.rd[("dma", kid)] = oid
        return oid

    def barrier(self):
        last = set()
        for e in self.ENG:
            for oid in reversed(self.streams[e]):
                if not self.ops[oid].is_dma:
                    last.add(oid)
                    break
        last.update(self.key_last.values())
        for e in self.ENG:
            self.barrier_deps[e] = set(last)
        self.phase += 1

    def emit(self):
        nc = self.nc
        ops = self.ops
        for op in ops:
            for d in op.deps:
                ops[d].signal = True
        nsem = [0]

        def newsem():
            nsem[0] += 1
            return self.stack.enter_context(nc.semaphore(f"s{nsem[0]}"))

        for eng in self.ENG:
            cur = None
            cnt = 0
            for oid in self.streams[eng]:
                op = ops[oid]
                if op.is_dma or not op.signal:
                    continue
                if cur is None or cnt >= SEM_LIMIT:
                    cur = newsem()
                    cnt = 0
                cnt += 1
                op.sem = cur
                op.count = cnt
        kstate = {}
        freepool = {True: [], False: []}
        kq = {}
        curphase = 0
        for op in ops:
            if not op.is_dma:
                continue
            if op.phase != curphase:
                curphase = op.phase
                for kk, st in kstate.items():
                    if st[1] + 16 <= SEM_LIMIT:
                        freepool[kq[kk]].append(st)
                kstate = {}
            kid = id(op.key)
            st = kstate.get(kid)
            if st is None or st[1] + 16 > SEM_LIMIT:
                sw = (op.eng == "pool")
                st = freepool[sw].pop() if freepool[sw] else [newsem(), 0]
                kstate[kid] = st
                kq[kid] = sw
            st[1] += 16
            op.sem = st[0]
            op.count = st[1]
        self.nsem = nsem[0]

        with nc.Block() as block:
            for eng in self.ENG:
                stream = self.streams[eng]
                if not stream:
                    continue

                def body(e, eng=eng, stream=stream):
                    waited = {}
                    for oid in stream:
                        op = ops[oid]
                        for d in sorted(op.deps):
                            dop = ops[d]
                            if dop.eng == eng and not dop.is_dma:
                                if eng == "pe" or not SAME_ENGINE_SYNC:
                                    continue
                            sid = id(dop.sem)
                            if waited.get(sid, 0) >= dop.count:
                                continue
                            e.wait_ge(dop.sem, dop.count)
                            waited[sid] = dop.count
                        ins = op.fn(e)
                        if op.signal:
                            ins.then_inc(op.sem, 16 if op.is_dma else 1)

                {"pe": block.tensor, "act": block.scalar, "dve": block.vector,
                 "pool": block.gpsimd, "sp": block.sync}[eng](body)


def _arena_init(self, nbytes):
    self.arena = self.stack.enter_context(self.nc.sbuf_tensor("arena", [128, nbytes // 4], F32))
    self.aoff = 0
    self.amax = nbytes // 4


def _arena_reset(self):
    self.barrier()
    self.aoff = 0


def _alloc(self, shape, dtype, name=""):
    esz = 2 if dtype == BF16 else 4
    nfree = 1
    for s in shape[1:]:
        nfree *= s
    nwords = (nfree * esz + 3) // 4
    nwords_al = (nwords + 15) // 16 * 16
    off = self.aoff
    assert off + nwords_al <= self.amax, f"arena overflow {name} {off} {nwords_al} {self.amax}"
    self.aoff += nwords_al
    ap = self.arena[0:shape[0], off:off + nwords]
    if dtype != F32:
        ap = ap.bitcast(dtype)
        if ap.shape[1] != nfree:
            ap = ap[:, 0:nfree]
    if len(shape) == 3:
        ap = ap.rearrange("p (a b) -> p a b", b=shape[2])
    elif len(shape) == 4:
        ap = ap.rearrange("p (a b c) -> p a b c", b=shape[2], c=shape[3])
    return Buf(ap, name)


Sched.arena_init = _arena_init
Sched.arena_reset = _arena_reset
Sched.alloc = _alloc


class Ring:
    def __init__(self, bufs):
        self.b = bufs
        self.i = 0

    def next(self):
        b = self.b[self.i % len(self.b)]
        self.i += 1
        return b


D = 2048
EPS = 1e-6

class _RingOld:
    def __init__(self, bufs): self.b = bufs; self.i = 0
    def next(self):
        b = self.b[self.i % len(self.b)]; self.i += 1; return b

def phase_A(S, C, T):
    nc = S.nc
    NB = T // 512
    x, w_in = C["x"], C["w_in"]
    psF, psT = C["psF"], C["psT"]
    ident = C["ident_bf"]
    gmix = S.alloc([128, D], F32, "gmix")
    S.dma(gmix[:], C["mix_g"][:], [], [gmix], gmix)
    gqk = S.alloc([128, 2], F32, "gqk")
    S.dma(gqk[:], C["qk_g"][:], [], [gqk], gqk)
    S.op("dve", lambda e: e.tensor_scalar(out=gqk[:, 0:1], in0=gqk[:, 0:1], scalar1=float(128 ** -0.5), scalar2=None, op0=ALU.mult), [gqk], [gqk])
    epsD = S.alloc([128, 1], F32, "epsD")
    S.op("dve", lambda e: e.memset(epsD[:], EPS), [], [epsD])
    ones_f = S.alloc([128, 128], F32, "ones_f")
    S.op("dve", lambda e: e.memset(ones_f[:], 1.0 / 128.0), [], [ones_f])
    xring = Ring([S.alloc([128, D], F32, f"xt{i}") for i in range(2)])
    hring = Ring([S.alloc([128, D], BF16, f"h{i}") for i in range(2)])
    junk = S.alloc([128, D], BF16, "junkA")
    hTring = Ring([S.alloc([128, 16, 512], BF16, f"hT{i}") for i in range(2)])
    wring = Ring([S.alloc([128, 16, 256], BF16, f"w{i}") for i in range(3)])
    sring = Ring([S.alloc([128, 1], F32, f"ss{i}") for i in range(4)])
    sqring = Ring([S.alloc([128, 512], F32, f"sq{i}") for i in range(2)])
    rsring = Ring([S.alloc([128, 512], F32, f"rs{i}") for i in range(2)])
    obring = Ring([S.alloc([128, 512], BF16, f"ob{i}") for i in range(4)])
    ofring = Ring([S.alloc([128, 512], F32, f"of{i}") for i in range(4)])
    pring = Ring(psF[0:5])
    p1ring = Ring(psF[5:7])
    w_v = w_in.t.rearrange("(kc p) f -> p kc f", p=128)
    pgen = peer_conv_gen(S, C)
    nconv = 128
    for tb in range(NB):
        hT = hTring.next()
        for tl in range(4):
            r0 = tb * 512 + tl * 128
            xt = xring.next(); h = hring.next(); ss = sring.next()
            S.dma(xt[:], x[r0:r0 + 128, :], [], [xt], xt)
            S.op("act", lambda e, xt=xt, ss=ss: e.activation(out=junk[:], in_=xt[:], func=AF.Square, accum_out=ss[:]), [xt], [junk, ss])
            S.op("act", lambda e, ss=ss: e.activation(out=ss[:], in_=ss[:], func=AF.Sqrt, scale=1.0 / D, bias=epsD[:]), [ss, epsD], [ss])
            S.op("dve", lambda e, ss=ss: e.reciprocal(out=ss[:], in_=ss[:]), [ss], [ss])
            S.op("dve", lambda e, xt=xt, ss=ss, h=h: e.scalar_tensor_tensor(out=h[:], in0=xt[:], scalar=ss[:, 0:1], in1=gmix[:], op0=ALU.mult, op1=ALU.mult), [xt, ss, gmix], [h])
            for half in range(2):
                for j in range(8):
                    kc = half * 8 + j
                    S.op("pe", lambda e, h=h, kc=kc, j=j: e.transpose(out=psT[:, j * 128:(j + 1) * 128], in_=h[:, kc * 128:(kc + 1) * 128], identity=ident[:]), [h, ident], [psT])
                S.op("act", lambda e, hT=hT, half=half, tl=tl: e.activation(out=hT[:, half * 8:half * 8 + 8, tl * 128:(tl + 1) * 128], in_=psT[:].rearrange("p (a b) -> p a b", b=128), func=AF.Copy), [psT], [hT])
        c0 = tb * 512
        for fg in range(32):
            if (fg % 2 == 0) and True:
                for _ in range(max(1, nconv // (NB * 16))):
                    next(pgen, None)
            wt = wring.next()
            S.dma(wt[:], C["w_in_bf"][fg], [], [wt], wt)
            if fg in (10, 11):
                for tl in range(4):
                    pv = pring.next()
                    for kc in range(16):
                        S.op("pe", lambda e, pv=pv, hT=hT, wt=wt, kc=kc, tl=tl: e.matmul(pv[:, 0:256], lhsT=hT[:, kc, tl * 128:(tl + 1) * 128], rhs=wt[:, kc, :], start=(kc == 0), stop=(kc == 15)), [hT, wt], [pv])
                    ob = obring.next()
                    S.op("act", lambda e, pv=pv, ob=ob: e.activation(out=ob[:, 0:256], in_=pv[:, 0:256], func=AF.Copy), [pv], [ob])
                    r0 = c0 + tl * 128
                    S.dma(C["v"][r0:r0 + 128, (fg - 10) * 256:(fg - 9) * 256], ob[:, 0:256], [ob], [], ob, queue=C.get("stq", "act"))
                continue
            for c2 in range(2):
                fc = fg * 2 + c2
                pq = pring.next()
                for kc in range(16):
                    S.op("pe", lambda e, pq=pq, hT=hT, wt=wt, kc=kc, c2=c2: e.matmul(pq[:], lhsT=wt[:, kc, c2 * 128:(c2 + 1) * 128], rhs=hT[:, kc, :], start=(kc == 0), stop=(kc == 15)), [hT, wt], [pq])
                if fc < 20:
                    sq = sqring.next(); rs = rsring.next(); p1 = p1ring.next(); ob = obring.next()
                    S.op("act", lambda e, pq=pq, sq=sq: e.activation(out=sq[:], in_=pq[:], func=AF.Square), [pq], [sq])
                    S.op("pe", lambda e, p1=p1, sq=sq: e.matmul(p1[:], lhsT=ones_f[:], rhs=sq[:], start=True, stop=True), [ones_f, sq], [p1])
                    S.op("act", lambda e, p1=p1, rs=rs: e.activation(out=rs[:], in_=p1[:], func=AF.Sqrt, bias=epsD[:]), [p1, epsD], [rs])
                    S.op("dve", lambda e, rs=rs: e.reciprocal(out=rs[:], in_=rs[:]), [rs], [rs])
                    gi = 0 if fc < 16 else 1
                    S.op("dve", lambda e, pq=pq, rs=rs, ob=ob, gi=gi: e.scalar_tensor_tensor(out=ob[:], in0=pq[:], scalar=gqk[:, gi:gi + 1], in1=rs[:], op0=ALU.mult, op1=ALU.mult), [pq, rs, gqk], [ob])
                    dst = C["qT"][fc, :, c0:c0 + 512] if fc < 16 else C["kT"][fc - 16, :, c0:c0 + 512]
                    S.dma(dst, ob[:], [ob], [], ob, queue=C.get("stq", "act"))
                elif fc < 32:
                    ob = obring.next()
                    S.op("act", lambda e, pq=pq, ob=ob: e.activation(out=ob[:], in_=pq[:], func=AF.Copy), [pq], [ob])
                    S.dma(C["uT"][fc - 24, :, c0:c0 + 512], ob[:], [ob], [], ob, queue=C.get("stq", "act"))
                else:
                    of = ofring.next()
                    S.op("act", lambda e, pq=pq, of=of: e.activation(out=of[:], in_=pq[:], func=AF.Sigmoid), [pq], [of])
                    dst = C["gaT"][fc - 32, :, c0:c0 + 512] if fc < 48 else C["gbT"][fc - 48, :, c0:c0 + 512]
                    S.dma(dst, of[:], [of], [], of, queue=C.get("stq", "act"))
    for _ in pgen:
        pass


PI = math.pi
SINSC = 1.0 - 2e-6


def bc3(ap2, n):
    return ap2.unsqueeze(2).to_broadcast([ap2.shape[0], ap2.shape[1], n])


def sincos(S, A, TH, shape, nm):
    KI = A(shape, I32, nm + "ki"); KF = A(shape, F32, nm + "kf"); ARG = A(shape, F32, nm + "arg")
    SN = A(shape, F32, nm + "sn"); CS = A(shape, F32, nm + "cs")
    S.op("dve", lambda e: e.tensor_scalar(out=KI[:], in0=TH[:], scalar1=1.0 / (2 * PI), scalar2=None, op0=ALU.mult), [TH], [KI])
    S.op("dve", lambda e: e.tensor_copy(out=KF[:], in_=KI[:]), [KI], [KF])
    S.op("dve", lambda e: e.scalar_tensor_tensor(out=ARG[:], in0=KF[:], scalar=-2 * PI, in1=TH[:], op0=ALU.mult, op1=ALU.add), [KF, TH], [ARG])
    S.op("act", lambda e: e.activation(out=SN[:], in_=ARG[:], func=AF.Sin, scale=SINSC), [ARG], [SN])
    S.op("dve", lambda e: e.tensor_scalar(out=KF[:], in0=ARG[:], scalar1=PI / 2, scalar2=-2 * PI, op0=ALU.is_gt, op1=ALU.mult), [ARG], [KF])
    S.op("dve", lambda e: e.scalar_tensor_tensor(out=ARG[:], in0=ARG[:], scalar=PI / 2, in1=KF[:], op0=ALU.add, op1=ALU.add), [ARG, KF], [ARG])
    S.op("act", lambda e: e.activation(out=CS[:], in_=ARG[:], func=AF.Sin, scale=SINSC), [ARG], [CS])
    return SN, CS


def phase_C0(S, C):
    A = S.alloc
    sh = [128, 128]
    TH = C["TH"] = S.sbuf(sh, F32, "TH"); R = C["R"] = S.sbuf(sh, F32, "Rdec"); TH2 = C["TH2"] = S.sbuf(sh, F32, "TH2")
    aim = A(sh, F32); are = A(sh, F32); ls = A(sh, F32); sgn = A([128, 2], F32)
    S.dma(aim[:], C["s_aim"][:], [], [aim], aim)
    S.dma(are[:], C["s_are"][:], [], [are], are)
    S.dma(ls[:], C["s_ls"][:], [], [ls], ls)
    S.dma(sgn[:], C["s_sgn"][:], [], [sgn], sgn)
    S.op("act", lambda e: e.activation(out=ls[:], in_=ls[:], func=AF.Exp), [ls], [ls])
    S.op("dve", lambda e: e.tensor_tensor(out=TH[:], in0=aim[:], in1=ls[:], op=ALU.mult), [aim, ls], [TH])
    S.op("dve", lambda e: e.tensor_scalar(out=TH2[:], in0=TH[:], scalar1=1.0 / (2 * PI), scalar2=None, op0=ALU.mult), [TH], [TH2])
    AR = A(sh, F32)
    S.op("dve", lambda e: e.tensor_tensor(out=AR[:], in0=are[:], in1=ls[:], op=ALU.mult), [are, ls], [AR])
    S.op("act", lambda e: e.activation(out=R[:], in_=AR[:], func=AF.Exp), [AR], [R])
    SN, CS = sincos(S, A, TH, sh, "c0")
    nr = A(sh, F32); ni = A(sh, F32); t1 = A(sh, F32); t2 = A(sh, F32); cr = A(sh, F32); ci = A(sh, F32)
    tt = lambda o, a, b, op, rd: S.op("dve", lambda e: e.tensor_tensor(out=o[:], in0=a[:], in1=b[:], op=op), rd, [o])
    tt(nr, R, CS, ALU.mult, [R, CS])
    S.op("dve", lambda e: e.tensor_scalar(out=nr[:], in0=nr[:], scalar1=-1.0, scalar2=None, op0=ALU.add), [nr], [nr])
    tt(ni, R, SN, ALU.mult, [R, SN])
    tt(t1, are, are, ALU.mult, [are])
    tt(t2, aim, aim, ALU.mult, [aim])
    tt(t1, t1, t2, ALU.add, [t1, t2])
    S.op("dve", lambda e: e.reciprocal(out=t1[:], in_=t1[:]), [t1], [t1])
    tt(cr, nr, are, ALU.mult, [nr, are])
    tt(t2, ni, aim, ALU.mult, [ni, aim])
    tt(cr, cr, t2, ALU.add, [cr, t2])
    tt(cr, cr, t1, ALU.mult, [cr, t1])
    tt(ci, ni, are, ALU.mult, [ni, are])
    tt(t2, nr, aim, ALU.mult, [nr, aim])
    tt(ci, ci, t2, ALU.subtract, [ci, t2])
    tt(ci, ci, t1, ALU.mult, [ci, t1])
    cis1 = A(sh, F32); crs2 = A(sh, F32)
    S.op("dve", lambda e: e.tensor_scalar(out=cis1[:], in0=ci[:], scalar1=sgn[:, 0:1], scalar2=None, op0=ALU.mult), [ci, sgn], [cis1])
    S.op("dve", lambda e: e.tensor_scalar(out=crs2[:], in0=cr[:], scalar1=sgn[:, 0:1], scalar2=-1.0, op0=ALU.mult, op1=ALU.mult), [cr, sgn], [crs2])
    sh3 = [128, 128, 16]
    X1 = A(sh3, F32); X2 = A(sh3, F32); LA = A(sh3, F32); LB = A(sh3, F32)
    PRM = A([128, 128, 4, 16], BF16)
    S.dma(X1[:], C["s_x1"][:], [], [X1], X1)
    S.dma(X2[:], C["s_x2"][:], [], [X2], X2)
    ttb = lambda o, a, b2, rd: S.op("dve", lambda e: e.tensor_tensor(out=o[:], in0=a[:], in1=bc3(b2[:], 16), op=ALU.mult), rd, [o])
    ttb(LA, X1, cr, [X1, cr]); ttb(LB, X2, cis1, [X2, cis1])
    S.op("dve", lambda e: e.tensor_tensor(out=PRM[:, :, 0, :], in0=LA[:], in1=LB[:], op=ALU.add), [LA, LB], [PRM])
    ttb(LA, X2, crs2, [X2, crs2]); ttb(LB, X1, ci, [X1, ci])
    S.op("dve", lambda e: e.tensor_tensor(out=PRM[:, :, 1, :], in0=LA[:], in1=LB[:], op=ALU.add), [LA, LB], [PRM])
    S.dma(X1[:], C["s_ca"][:], [], [X1], X1)
    S.dma(X2[:], C["s_cb"][:], [], [X2], X2)
    S.op("dve", lambda e: e.tensor_scalar(out=PRM[:, :, 2, :], in0=X1[:], scalar1=sgn[:, 1:2], scalar2=None, op0=ALU.mult), [X1, sgn], [PRM])
    S.op("dve", lambda e: e.tensor_scalar(out=PRM[:, :, 3, :], in0=X2[:], scalar1=-1.0, scalar2=None, op0=ALU.mult), [X2], [PRM])
    S.dma(C["prm"][:], PRM[:].rearrange("p a b c -> p a (b c)"), [PRM], [], PRM)


def phase_C1(S, C, NSEQ, SEQ):
    A = S.alloc
    T = NSEQ * SEQ
    NP = SEQ // 512
    psF, psT, ident = C["psF"], C["psT"], C["ident_bf"]
    TH, R, TH2 = C["TH"], C["R"], C["TH2"]
    iota = A([128, SEQ], F32, "iota")
    S.dma(iota[:], C["iota"][:, 0:SEQ], [], [iota], iota)
    dcol = A([128, 8], F32, "dcol")
    S.dma(dcol[:], C["ssm_d"][:], [], [dcol], dcol)
    hpi = A([128, 1], F32, "hpi")
    S.op("dve", lambda e: e.memset(hpi[:], PI / 2), [], [hpi])
    HS = min(1024, SEQ)
    NH = SEQ // HS
    COSr = Ring([A([128, SEQ], F32, f"COS{i}") for i in range(3)])
    SINr = Ring([A([128, SEQ], F32, f"SIN{i}") for i in range(3)])
    magic = A([128, 1], F32, "magic"); nmagic = A([128, 1], F32, "nmagic")
    S.op("dve", lambda e: e.memset(magic[:], 12582912.0), [], [magic])
    S.op("dve", lambda e: e.memset(nmagic[:], -12582912.0), [], [nmagic])
    KFs = [A([128, HS], F32, "KF0")] * NH
    ARGs = [A([128, HS], F32, "ARG0")] * NH
    ABs = [A([128, HS], F32, "AB0")] * NH
    Wb = [A([128, SEQ], F32, f"W{i}") for i in range(2)]
    Vb = [A([128, SEQ], F32, f"V{i}") for i in range(2)]
    PCr = Ring([A([128, SEQ], BF16, f"PC{i}") for i in range(2 * NSEQ)])
    PSr = Ring([A([128, SEQ], BF16, f"PSb{i}") for i in range(2 * NSEQ)])
    yacc = A([128, T], F32, "yacc")
    yst = A([128, T], BF16, "yst")
    uch = A([128, T], BF16, "uch")
    ugr = [A([128, T], BF16, f"ug{i}") for i in range(2)]
    for ug in ugr:
        S.op("pool", lambda e, ug=ug: e.memset(ug[:], 0.0), [], [ug])
    ugring = Ring(ugr)
    Lr = [A([128, 2, 128], BF16, f"L{i}") for i in range(2)]
    for L in Lr:
        S.op("pool", lambda e, L=L: e.memset(L[:], 0.0), [], [L])
    Lring = Ring(Lr)
    CABring = Ring([A([128, 2, 128], BF16, f"CAB{i}") for i in range(2)])
    prmring = Ring([A([128, 64], BF16, f"prm{i}") for i in range(3)])
    tAr = Ring([A([128, 512], F32, f"tA{i}") for i in range(2)])
    tBr = Ring([A([128, 512], F32, f"tB{i}") for i in range(2)])
    bur = Ring(psF[0:4])
    pyr = Ring(psF[4:7])
    iters = [(cc, gi, d) for cc in range(8) for gi in range(8) for d in range(2)]
    state = {}

    tabs = {}

    def tables(it):
        cc, gi, d = it
        dg = d * 64 + cc * 8 + gi
        COS = COSr.next(); SIN = SINr.next()
        tabs[it] = (COS, SIN)
        for hs in range(NH):
            sl = slice(hs * HS, (hs + 1) * HS)
            S.op("act", lambda e, sl=sl, dg=dg, hs=hs: e.activation(out=KFs[hs][:], in_=iota[:, sl], func=AF.Identity, scale=TH2[:, dg:dg + 1], bias=magic[:]), [iota, TH2, magic], [KFs[hs]])
            S.op("act", lambda e, hs=hs: e.activation(out=KFs[hs][:], in_=KFs[hs][:], func=AF.Identity, bias=nmagic[:]), [KFs[hs], nmagic], [KFs[hs]])
            S.op("dve", lambda e, sl=sl, dg=dg, hs=hs: e.scalar_tensor_tensor(out=ARGs[hs][:], in0=iota[:, sl], scalar=TH2[:, dg:dg + 1], in1=KFs[hs][:], op0=ALU.mult, op1=ALU.subtract), [iota, TH2, KFs[hs]], [ARGs[hs]])
            S.op("act", lambda e, sl=sl, hs=hs, SIN=SIN: e.activation(out=SIN[:, sl], in_=ARGs[hs][:], func=AF.Sin, scale=2 * PI * SINSC), [ARGs[hs]], [SIN])
            S.op("act", lambda e, hs=hs: e.activation(out=ABs[hs][:], in_=ARGs[hs][:], func=AF.Abs), [ARGs[hs]], [ABs[hs]])
            S.op("act", lambda e, sl=sl, hs=hs, COS=COS: e.activation(out=COS[:, sl], in_=ABs[hs][:], func=AF.Sin, scale=-2 * PI * SINSC, bias=hpi[:]), [ABs[hs], hpi], [COS])


    def head(it):
        cc, gi, d = it
        g = cc * 8 + gi
        dg = d * 64 + g
        if gi == 0 and d == 0:
            S.dma(uch[:], C["uT"][cc, :, :], [], [uch], uch)
        if d == 0:
            ug = ugring.next()
            S.dma(ug[0:16, :], C["uT"][cc, gi * 16:(gi + 1) * 16, :], [], [ug], ug)
            state["ug"] = ug
        ug = state["ug"]
        prm = prmring.next(); L = Lring.next(); CAB = CABring.next()
        S.dma(prm[:], C["prm"][:, dg, :], [], [prm], prm)
        for j in range(2):
            S.op("pe", lambda e, prm=prm, j=j: e.transpose(out=psT[0:16, j * 128:(j + 1) * 128], in_=prm[:, j * 16:(j + 1) * 16], identity=ident[:]), [prm, ident], [psT])
        S.op("act", lambda e, L=L: e.activation(out=L[0:16, :, :], in_=psT[0:16, 0:256].rearrange("p (a b) -> p a b", b=128), func=AF.Copy), [psT], [L])
        S.op("pool", lambda e, CAB=CAB: e.memset(CAB[:], 0.0), [], [CAB])
        S.op("pool", lambda e, CAB=CAB, prm=prm, gi=gi: e.tensor_copy(out=CAB[:, :, gi * 16:(gi + 1) * 16], in_=prm[:, 32:64].rearrange("p (a b) -> p a b", b=16)), [prm], [CAB])
        COS, SIN = tabs.pop(it)
        def tsl(ap, p, d=d):
            if d == 0:
                return ap[:, p * 512:(p + 1) * 512]
            return ap[:, SEQ - (p + 1) * 512:SEQ - p * 512][:, ::-1]
        for b in range(NSEQ):
            W = Wb[b]
            for p in range(NP):
                c0 = b * SEQ + p * 512
                b1 = bur.next(); b2 = bur.next(); tA = tAr.next(); tB = tBr.next()
                S.op("pe", lambda e, b1=b1, L=L, ug=ug, c0=c0: e.matmul(b1[:], lhsT=L[:, 0, :], rhs=ug[:, c0:c0 + 512], start=True, stop=True), [L, ug], [b1])
                S.op("pe", lambda e, b2=b2, L=L, ug=ug, c0=c0: e.matmul(b2[:], lhsT=L[:, 1, :], rhs=ug[:, c0:c0 + 512], start=True, stop=True), [L, ug], [b2])
                S.op("dve", lambda e, tA=tA, b1=b1, p=p, tsl=tsl, COS=COS: e.tensor_tensor(out=tA[:], in0=b1[:], in1=tsl(COS, p), op=ALU.mult), [b1, COS], [tA])
                S.op("dve", lambda e, tB=tB, b2=b2, p=p, tsl=tsl, SIN=SIN: e.tensor_tensor(out=tB[:], in0=b2[:], in1=tsl(SIN, p), op=ALU.mult), [b2, SIN], [tB])
                S.op("pool", lambda e, tA=tA, tB=tB, p=p, W=W: e.tensor_tensor(out=W[:, p * 512:(p + 1) * 512], in0=tA[:], in1=tB[:], op=ALU.add), [tA, tB], [W])
        pcs = []
        for b in range(NSEQ):
            W = Wb[b]; V = Vb[b]; PC = PCr.next(); PS = PSr.next()
            if d == 0:
                S.op("dve", lambda e, dg=dg, W=W, V=V: e.tensor_tensor_scan(out=V[:], data0=R[:, dg:dg + 1].to_broadcast([128, SEQ]), data1=W[:], initial=0.0, op0=ALU.mult, op1=ALU.add), [R, W], [V])
                S.op("pool", lambda e, V=V, PC=PC, COS=COS: e.tensor_tensor(out=PC[:], in0=V[:], in1=COS[:], op=ALU.mult), [V, COS], [PC])
                S.op("pool", lambda e, V=V, PS=PS, SIN=SIN: e.tensor_tensor(out=PS[:], in0=V[:], in1=SIN[:], op=ALU.mult), [V, SIN], [PS])
            else:
                S.op("dve", lambda e, dg=dg, W=W, V=V: e.tensor_tensor_scan(out=V[:, ::-1], data0=R[:, dg:dg + 1].to_broadcast([128, SEQ]), data1=W[:, ::-1], initial=0.0, op0=ALU.mult, op1=ALU.add), [R, W], [V])
                S.op("pool", lambda e, V=V, PC=PC, COS=COS: e.tensor_tensor(out=PC[:], in0=V[:], in1=COS[:, ::-1], op=ALU.mult), [V, COS], [PC])
                S.op("pool", lambda e, V=V, PS=PS, SIN=SIN: e.tensor_tensor(out=PS[:], in0=V[:], in1=SIN[:, ::-1], op=ALU.mult), [V, SIN], [PS])
            pcs.append((PC, PS))
        return (CAB, pcs)

    def tail(it, hd):
        cc, gi, d = it
        CAB, pcs = hd
        if gi == 0 and d == 0:
            S.op("dve", lambda e, cc=cc: e.tensor_scalar(out=yacc[:], in0=uch[:], scalar1=dcol[:, cc:cc + 1], scalar2=None, op0=ALU.mult), [uch, dcol], [yacc])
        for b in range(NSEQ):
            PC, PS = pcs[b]
            for p in range(NP):
                c0 = b * SEQ + p * 512
                py = pyr.next()
                S.op("pe", lambda e, py=py, CAB=CAB, p=p, PC=PC: e.matmul(py[:], lhsT=CAB[:, 0, :], rhs=PC[:, p * 512:(p + 1) * 512], start=True, stop=False), [CAB, PC], [py])
                S.op("pe", lambda e, py=py, CAB=CAB, p=p, PS=PS: e.matmul(py[:], lhsT=CAB[:, 1, :], rhs=PS[:, p * 512:(p + 1) * 512], start=False, stop=True), [CAB, PS], [py])
                S.op("dve", lambda e, py=py, c0=c0: e.tensor_tensor(out=yacc[:, c0:c0 + 512], in0=py[:], in1=yacc[:, c0:c0 + 512], op=ALU.add), [py, yacc], [yacc])
        if gi == 7 and d == 1:
            S.op("act", lambda e: e.activation(out=yst[:], in_=yacc[:], func=AF.Gelu), [yacc], [yst])
            S.dma(C["yact"][cc, :, :], yst[:], [yst], [], yst, queue="act")

    tables(iters[0])
    if len(iters) > 1:
        tables(iters[1])
    hd = head(iters[0])
    for i, it in enumerate(iters):
        if i + 2 < len(iters):
            tables(iters[i + 2])
        nhd = head(iters[i + 1]) if i + 1 < len(iters) else None
        tail(it, hd)
        hd = nhd


def phase_C2(S, C, T):
    A = S.alloc
    psF = C["psF"]
    YACT = A([128, 8, T], BF16, "YACT")
    for cc in range(8):
        S.dma(YACT[:, cc, :], C["yact"][cc, :, :], [], [YACT], YACT)
    war = Ring([A([128, 8, 128], BF16, f"wa{i}") for i in range(2)])
    wbr = Ring([A([128, 8, 128], BF16, f"wb{i}") for i in range(2)])
    sbr = Ring([A([128, 512], F32, f"sb{i}") for i in range(2)])
    gbr = Ring([A([128, 512], F32, f"gbt{i}") for i in range(2)])
    obr = Ring([A([128, 512], F32, f"mbo{i}") for i in range(3)])
    par = Ring(psF[0:3]); pbr = Ring(psF[3:6])
    wa_v = C["glu_a"].t.rearrange("(kc p) f -> p kc f", p=128)
    wb_v = C["glu_b"].t.rearrange("(kc p) f -> p kc f", p=128)
    for dc in range(16):
        wa = war.next(); wb = wbr.next()
        S.dma(wa[:], C["glu_a_bf"][dc], [], [wa], wa)
        S.dma(wb[:], C["glu_b_bf"][dc], [], [wb], wb)
        for p in range(T // 512):
            c0 = p * 512
            pa = par.next(); pb = pbr.next(); sb = sbr.next(); gbt = gbr.next(); ob = obr.next()
            S.dma(gbt[:], C["gbT"][dc, :, c0:c0 + 512], [], [gbt], gbt)
            for kc in range(8):
                S.op("pe", lambda e, pa=pa, wa=wa, kc=kc, c0=c0: e.matmul(pa[:], lhsT=wa[:, kc, :], rhs=YACT[:, kc, c0:c0 + 512], start=(kc == 0), stop=(kc == 7)), [wa, YACT], [pa])
            for kc in range(8):
                S.op("pe", lambda e, pb=pb, wb=wb, kc=kc, c0=c0: e.matmul(pb[:], lhsT=wb[:, kc, :], rhs=YACT[:, kc, c0:c0 + 512], start=(kc == 0), stop=(kc == 7)), [wb, YACT], [pb])
            S.op("act", lambda e, pb=pb, sb=sb: e.activation(out=sb[:], in_=pb[:], func=AF.Sigmoid), [pb], [sb])
            S.op("dve", lambda e, pa=pa, sb=sb: e.tensor_tensor(out=sb[:], in0=pa[:], in1=sb[:], op=ALU.mult), [pa, sb], [sb])
            S.op("pool", lambda e, sb=sb, gbt=gbt, ob=ob: e.tensor_tensor(out=ob[:], in0=sb[:], in1=gbt[:], op=ALU.mult), [sb, gbt], [ob])
            S.dma(C["MB"][dc, :, c0:c0 + 512], ob[:], [ob], [], ob, queue="act")


D = 2048
EPS = 1e-6


def v4(ap):
    return ap.rearrange("p (a b) -> p a b", b=128)


def phase_B(S, C, NSEQ, SEQ):
    A = S.alloc
    T = NSEQ * SEQ
    NBS = SEQ // 512
    NKB = SEQ // 128
    psF, psT, ident = C["psF"], C["psT"], C["ident_bf"]
    bias = A([128, 3, 16, 128], F32, "bias")
    S.dma(bias[:], C["alibi"][:].rearrange("p (j h t) -> p j h t", j=3, h=16), [], [bias], bias)
    esink = A([128, 16], F32, "esink")
    S.dma(esink[:], C["sink_b"][:], [], [esink], esink)
    S.op("act", lambda e: e.activation(out=esink[:], in_=esink[:], func=AF.Exp), [esink], [esink])
    ones_bf = A([128, 128], BF16, "ones_bf")
    S.op("dve", lambda e: e.memset(ones_bf[:], 1.0), [], [ones_bf])
    g2 = A([128, D], F32, "g2")
    S.dma(g2[:], C["ffn_g"][:], [], [g2], g2)
    epsD = A([128, 1], F32, "epsB")
    S.op("dve", lambda e: e.memset(epsD[:], EPS), [], [epsD])
    junk = A([128, D], BF16, "junkB")
    kring = Ring([A([128, 4, 768], BF16, f"kTb{i}") for i in range(2)])
    vring = Ring([A([128, 6, 512], BF16, f"vt{i}") for i in range(2)])
    qring = Ring([A([128, 4, 512], BF16, f"qTb{i}") for i in range(2)])
    etring = Ring([A([128, 3, 512], BF16, f"ET{i}") for i in range(2)])
    sbring = Ring([A([128, 512], F32, f"sbB{i}") for i in range(3)])
    dnring = Ring([A([128, 512], F32, f"dn{i}") for i in range(2)])
    OT = A([128, 16, 512], BF16, "OT")
    MT = A([128, 16, 512], BF16, "MT")
    woring = Ring([A([128, 16, 128], BF16, f"wo{i}") for i in range(2)])
    garing = Ring([A([128, 512], F32, f"gat{i}") for i in range(2)])
    mbring = Ring([A([128, 512], F32, f"mbt{i}") for i in range(2)])
    tmring = Ring([A([128, 512], F32, f"tm{i}") for i in range(2)])
    wtring = Ring([A([128, 16, 256], BF16, f"wout{i}") for i in range(2)])
    x1t = [A([128, D], F32, f"x1t{i}") for i in range(4)]
    h2ring = Ring([A([128, D], BF16, f"h2_{i}") for i in range(2)])
    h2Tb = OT
    ssring = Ring([A([128, 1], F32, f"ssB{i}") for i in range(4)])
    sring = Ring(psF[0:3])
    psO, psD = psF[3], psF[4]
    pring = Ring(psF[5:7])
    wo_v = C["w_o"].t.rearrange("(kc p) f -> p kc f", p=128)
    wout_v = C["w_out"].t.rearrange("(kc p) f -> p kc f", p=128)
    for tb in range(T // 512):
        b = tb // NBS; tbl = tb % NBS; c0 = tb * 512
        kTb = kring.next(); vt = vring.next()
        jbv = [0 <= 4 * tbl - 1 + jb < NKB for jb in range(6)]
        lo = jbv.index(True); hi = 6 - jbv[::-1].index(True)
        g0 = b * SEQ + (4 * tbl - 1 + lo) * 128; n = (hi - lo) * 128
        S.dma(kTb[:, :, lo * 128:hi * 128], C["kT"][:, :, g0:g0 + n].rearrange("k p t -> p k t"), [], [kTb], kTb)
        S.dma(vt[:, lo:hi, :], C["v"][g0:g0 + n, :].rearrange("(j p) f -> p j f", p=128), [], [vt], vt)
        for tl in range(4):
            r0 = c0 + tl * 128
            S.dma(x1t[tl][:], C["x"][r0:r0 + 128, :], [], [x1t[tl]], x1t[tl])
        qtbs = {}

        def stageS(kv, qb):
            if qb == 0:
                qTb = qring.next()
                S.dma(qTb[:], C["qT"][kv * 4:(kv + 1) * 4, :, c0:c0 + 512].rearrange("h p t -> p h t"), [], [qTb], qTb)
                qtbs[kv] = qTb
            qTb = qtbs[kv]
            js = [j for j in range(3) if jbv[qb + j]]
            ET = etring.next()
            for j in js:
                ps = sring.next(); sb = sbring.next()
                S.op("pe", lambda e, ps=ps, kTb=kTb, qTb=qTb, kv=kv, qb=qb, j=j: e.matmul(v4(ps[:]), lhsT=kTb[:, kv, (qb + j) * 128:(qb + j + 1) * 128], rhs=qTb[:, :, qb * 128:(qb + 1) * 128], start=True, stop=True), [kTb, qTb], [ps])
                S.op("dve", lambda e, ps=ps, sb=sb, j=j, kv=kv: e.tensor_tensor(out=v4(sb[:]), in0=v4(ps[:]), in1=bias[:, j, kv * 4:(kv + 1) * 4, :], op=ALU.add), [ps, bias], [sb])
                S.op("act", lambda e, sb=sb, ET=ET, j=j: e.activation(out=ET[:, j, :], in_=sb[:], func=AF.Exp), [sb], [ET])
            return (js, ET)

        def stagePV(kv, qb, st):
            js, ET = st
            for idx, j in enumerate(js):
                S.op("pe", lambda e, vt=vt, ET=ET, kv=kv, qb=qb, j=j, idx=idx, n=len(js): e.matmul(psO[:], lhsT=vt[:, qb + j, kv * 128:(kv + 1) * 128], rhs=ET[:, j, :], start=(idx == 0), stop=(idx == n - 1)), [vt, ET], [psO])
            for idx, j in enumerate(js):
                S.op("pe", lambda e, ET=ET, j=j, idx=idx, n=len(js): e.matmul(psD[:], lhsT=ones_bf[:], rhs=ET[:, j, :], start=(idx == 0), stop=(idx == n - 1)), [ones_bf, ET], [psD])
            dn = dnring.next()
            S.op("dve", lambda e, dn=dn, kv=kv: e.tensor_tensor(out=v4(dn[:]), in0=v4(psD[:]), in1=esink[:, kv * 4:(kv + 1) * 4].unsqueeze(2).to_broadcast([128, 4, 128]), op=ALU.add), [psD, esink], [dn])
            S.op("dve", lambda e, dn=dn: e.reciprocal(out=dn[:], in_=dn[:]), [dn], [dn])
            S.op("dve", lambda e, dn=dn, kv=kv, qb=qb: e.tensor_tensor(out=OT[:, kv * 4:(kv + 1) * 4, qb * 128:(qb + 1) * 128], in0=v4(psO[:]), in1=v4(dn[:]), op=ALU.mult), [psO, dn], [OT])

        groups = [(kv, qb) for kv in range(4) for qb in range(4)]
        stg = stageS(*groups[0])
        for gi_, g_ in enumerate(groups):
            nstg = stageS(*groups[gi_ + 1]) if gi_ + 1 < len(groups) else None
            stagePV(g_[0], g_[1], stg)
            stg = nstg
        for dc in range(16):
            wo = woring.next(); gat = garing.next(); mbt = mbring.next(); tm = tmring.next(); pa = pring.next()
            S.dma(wo[:], C["w_o_bf"][dc], [], [wo], wo)
            S.dma(gat[:], C["gaT"][dc, :, c0:c0 + 512], [], [gat], gat)
            S.dma(mbt[:], C["MB"][dc, :, c0:c0 + 512], [], [mbt], mbt)
            for h in range(16):
                S.op("pe", lambda e, pa=pa, wo=wo, h=h: e.matmul(pa[:], lhsT=wo[:, h, :], rhs=OT[:, h, :], start=(h == 0), stop=(h == 15)), [wo, OT], [pa])
            S.op("dve", lambda e, pa=pa, gat=gat, tm=tm: e.tensor_tensor(out=tm[:], in0=pa[:], in1=gat[:], op=ALU.mult), [pa, gat], [tm])
            S.op("pool", lambda e, tm=tm, mbt=mbt, dc=dc: e.tensor_tensor(out=MT[:, dc, :], in0=tm[:], in1=mbt[:], op=ALU.add), [tm, mbt], [MT])
        for cg in range(8):
            wt = wtring.next()
            S.dma(wt[:], C["w_out_bf"][cg], [], [wt], wt)
            for tl in range(4):
                px = pring.next()
                for kc in range(16):
                    S.op("pe", lambda e, px=px, wt=wt, kc=kc, tl=tl: e.matmul(px[:, 0:256], lhsT=MT[:, kc, tl * 128:(tl + 1) * 128], rhs=wt[:, kc, :], start=(kc == 0), stop=(kc == 15)), [MT, wt], [px])
                S.op("dve", lambda e, px=px, tl=tl, cg=cg: e.tensor_tensor(out=x1t[tl][:, cg * 256:(cg + 1) * 256], in0=px[:, 0:256], in1=x1t[tl][:, cg * 256:(cg + 1) * 256], op=ALU.add), [px, x1t[tl]], [x1t[tl]])
        for tl in range(4):
            r0 = c0 + tl * 128
            xt = x1t[tl]; ss = ssring.next(); h2 = h2ring.next()
            S.dma(C["x1"][r0:r0 + 128, :], xt[:], [xt], [], xt, queue="act")
            S.op("act", lambda e, xt=xt, ss=ss: e.activation(out=junk[:], in_=xt[:], func=AF.Square, accum_out=ss[:]), [xt], [junk, ss])
            S.op("act", lambda e, ss=ss: e.activation(out=ss[:], in_=ss[:], func=AF.Sqrt, scale=1.0 / D, bias=epsD[:]), [ss, epsD], [ss])
            S.op("dve", lambda e, ss=ss: e.reciprocal(out=ss[:], in_=ss[:]), [ss], [ss])
            S.op("dve", lambda e, xt=xt, ss=ss, h2=h2: e.scalar_tensor_tensor(out=h2[:], in0=xt[:], scalar=ss[:, 0:1], in1=g2[:], op0=ALU.mult, op1=ALU.mult), [xt, ss, g2], [h2])
            for half in range(2):
                for j in range(8):
                    kc = half * 8 + j
                    S.op("pe", lambda e, h2=h2, kc=kc, j=j: e.transpose(out=psT[:, j * 128:(j + 1) * 128], in_=h2[:, kc * 128:(kc + 1) * 128], identity=ident[:]), [h2, ident], [psT])
                S.op("act", lambda e, half=half, tl=tl: e.activation(out=h2Tb[:, half * 8:half * 8 + 8, tl * 128:(tl + 1) * 128], in_=v4(psT[:]), func=AF.Copy), [psT], [h2Tb])
        S.dma(C["h2T"][:, :, c0:c0 + 512], h2Tb[:], [h2Tb], [], h2Tb, queue="act")


D = 2048
NEG = -1.0e30
MARGIN = 1.0e-4


def phase_P0(S, C):
    A = S.alloc
    fr = Ring([A([128, 4096], F32, f"cvf{i}") for i in range(3)])
    br = Ring([A([128, 4096], BF16, f"cvb{i}") for i in range(3)])
    engs = ["act", "dve", "pool"]
    kk = [0]

    def cast(f, b, cw):
        en = engs[kk[0] % 3]; kk[0] += 1
        if en == "act":
            S.op("act", lambda e, f=f, b=b, cw=cw: e.activation(out=b[:, 0:cw], in_=f[:, 0:cw], func=AF.Copy), [f], [b])
        else:
            S.op(en, lambda e, f=f, b=b, cw=cw: e.tensor_copy(out=b[:, 0:cw], in_=f[:, 0:cw]), [f], [b])
    for src, dst, K, F, fw in ((C["w_in"], C["w_in_bf"], 2048, 8192, 256), (C["w_o"], C["w_o_bf"], 2048, 2048, 128),
                               (C["w_out"], C["w_out_bf"], 2048, 2048, 256), (C["glu_a"], C["glu_a_bf"], 1024, 2048, 128),
                               (C["glu_b"], C["glu_b_bf"], 1024, 2048, 128), (C["peer_wq"], C["wq_bf"], 2048, 2048, 128)):
        cw = min(F, 4096)
        for kc in range(K // 128):
            for c in range(F // cw):
                f = fr.next(); b = br.next()
                S.dma(f[:, 0:cw], src[kc * 128:(kc + 1) * 128, c * cw:(c + 1) * cw], [], [f], f)
                cast(f, b, cw)
                ng = cw // fw
                S.dma(dst[c * ng:(c + 1) * ng, :, kc, :].rearrange("g p f -> p g f"), b[:, 0:cw].rearrange("p (g f) -> p g f", f=fw), [b], [], b, queue="act")


def peer_conv_gen(S, C):
    A = S.alloc
    fr = Ring([A([128, 4096], F32, f"pcf{i}") for i in range(3)])
    br = Ring([A([128, 4096], BF16, f"pcb{i}") for i in range(3)])
    for src, dst, R, Cc in ((C["peer_downT"], C["downT_bf"], 2048, 16384), (C["peer_up"], C["up_bf"], 16384, 2048)):
        cw = min(Cc, 4096)
        rr = 4096 // cw
        for r in range(0, R // 128, rr):
            for c in range(Cc // cw):
                f = fr.next(); b = br.next()
                for q in range(rr):
                    S.dma(f[:, q * cw:(q + 1) * cw], src[(r + q) * 128:(r + q + 1) * 128, c * cw:(c + 1) * cw], [], [f], f)
                S.op("pool", lambda e, f=f, b=b: e.tensor_copy(out=b[:], in_=f[:]), [f], [b])
                for q in range(rr):
                    S.dma(dst[(r + q) * 128:(r + q + 1) * 128, c * cw:(c + 1) * cw], b[:, q * cw:(q + 1) * cw], [b], [], b, queue="act")
                yield


def phase_D(S, C, T):
    A = S.alloc
    psS, psBig, psT, ident = C["psS"], C["psBig"], C["psT"], C["ident_bf"]
    subk = A([128, 16, 128], F32, "subk")
    S.dma(subk[:], C["subk"][:], [], [subk], subk)
    h2Tb = A([128, 16, 512], BF16, "h2TbD")
    SC = A([128, 4, 16, 128], F32, "SC")
    SCv = SC[:].rearrange("p t (h s) k -> p t h s k", s=2)
    SCt = [Buf(SC[:, tl, :, :], f"SCt{tl}") for tl in range(4)]
    V16t = []; T16t = []; smalls = []
    for tl in range(4):
        Vb_ = A([128, 16, 16], F32, f"V16_{tl}")
        V16t.append({"t": Vb_, "v": Vb_[:].rearrange("p (h s) k -> p h s k", s=2),
                     "a": [Buf(Vb_[:, sg, 0:8]) for sg in range(16)], "b": [Buf(Vb_[:, sg, 8:16]) for sg in range(16)]})
        Tb_ = A([128, 8, 16], F32, f"T16_{tl}")
        T16t.append({"t": Tb_, "a": [Buf(Tb_[:, h, 0:8]) for h in range(8)], "b": [Buf(Tb_[:, h, 8:16]) for h in range(8)]})
        OFF_ = A([128, 16], F32, f"OFF{tl}")
        smalls.append({"OFF": OFF_, "OFFv": OFF_[:].rearrange("p (h s) -> p h s", s=2), "EX": A([128, 8, 16], F32, f"EX{tl}"),
                       "Z": A([128, 8], F32, f"Z{tl}"), "thrE": A([128, 8], F32, f"thrE{tl}")})
    tms = [A([128, 128], F32, f"tmk{i}") for i in range(8)] * 2
    cand = A([128, 8, 256], F32, "cand")
    cand4 = cand[:].rearrange("p h (a b) -> p h a b", b=16)
    tcs = [A([128, 256], F32, f"tmc{i}") for i in range(4)] * 2
    DG = [A([128, 8, 128], BF16, f"DG{i}") for i in range(4)]
    dTr = Ring([A([128, 16, 256], BF16, f"dT{i}") for i in range(2)])
    upr = Ring([A([128, 2, 2048], BF16, f"upc{i}") for i in range(2)])
    GLs = [A([128, 4, 256], BF16, f"GL{i}") for i in range(2)]
    Eps = [A([128, 8, 2, 128], F32, f"Ep{i}") for i in range(2)]
    Mp0 = A([128, 8, 256], BF16, "Mp0")
    Rp0 = A([128, 8, 256], BF16, "Rp0")
    Mk0 = A([128, 8, 256], BF16, "Mk0")
    neg1 = A([128, 1], F32, "neg1")
    S.op("dve", lambda e: e.memset(neg1[:], -1.0), [], [neg1])
    Wtr = Ring([A([128, 256], BF16, f"Wt{i}") for i in range(2)])
    WTr = Ring([A([128, 256], BF16, f"WT{i}") for i in range(2)])
    oacc = [A([128, D], F32, f"oacc{i}") for i in range(4)]
    oaccH = [[Buf(oacc[i][:, h * 1024:(h + 1) * 1024], f"oaccH{i}{h}") for h in range(2)] for i in range(4)]
    psBh = [Buf(psBig[:, h * 1024:(h + 1) * 1024], f"psBh{h}") for h in range(2)]
    wqr = Ring([A([128, 16, 128], BF16, f"wq{i}") for i in range(2)])
    qcr = Ring([A([128, 512], F32, f"qc{i}") for i in range(2)])
    par = Ring(psS[0:2])
    pg = psS[2]
    wq_v = C["peer_wq"].t.rearrange("(kc p) f -> p kc f", p=128)
    dT_v = C["downT_bf"].t.rearrange("(kc p) e -> p kc e", p=128)
    for tb in range(T // 512):
        c0 = tb * 512
        S.dma(h2Tb[:], C["h2T"][:, :, c0:c0 + 512], [], [h2Tb], h2Tb)
        for tl in range(4):
            S.dma(oacc[tl][:], C["x1"][c0 + tl * 128:c0 + (tl + 1) * 128, :], [], [oacc[tl]] + oaccH[tl], oacc[tl])
        for fch in range(16):
            wq = wqr.next(); qc = qcr.next(); pq = par.next()
            S.dma(wq[:], C["wq_bf"][fch], [], [wq], wq)
            for kc in range(16):
                S.op("pe", lambda e, pq=pq, wq=wq, kc=kc: e.matmul(pq[:], lhsT=wq[:, kc, :], rhs=h2Tb[:, kc, :], start=(kc == 0), stop=(kc == 15)), [wq, h2Tb], [pq])
            S.op("act", lambda e, pq=pq, qc=qc: e.activation(out=qc[:], in_=pq[:], func=AF.Copy), [pq], [qc])
            for tl in range(4):
                S.op("pe", lambda e, qc=qc, tl=tl, fch=fch: e.matmul(pg[:, tl * 128:(tl + 1) * 128], lhsT=qc[:, tl * 128:(tl + 1) * 128], rhs=subk[:, fch, :], start=True, stop=True), [qc, subk], [pg])
            S.op("act", lambda e, fch=fch: e.activation(out=SC[:, :, fch, :], in_=pg[:].rearrange("p (a b) -> p a b", b=128), func=AF.Copy), [pg], SCt)
        def batches(tl):
            V = V16t[tl]; Tt = T16t[tl]
            for sh in range(2):
                segs = range(sh * 8, sh * 8 + 8)
                for seg in segs:
                    S.op("dve", lambda e, tl=tl, seg=seg, V=V: e.max(out=V["a"][seg][:], in_=SC[:, tl, seg, :]), [SCt[tl]], [V["a"][seg]])
                for seg in segs:
                    S.op("dve", lambda e, tl=tl, seg=seg, V=V: e.match_replace(out=tms[seg][:], in_to_replace=V["a"][seg][:], in_values=SC[:, tl, seg, :], imm_value=NEG), [SCt[tl], V["a"][seg]], [tms[seg]])
                for seg in segs:
                    S.op("dve", lambda e, seg=seg, V=V: e.max(out=V["b"][seg][:], in_=tms[seg][:]), [tms[seg]], [V["b"][seg]])
            Vv = V["v"]
            S.op("dve", lambda e, Vv=Vv: e.tensor_tensor(out=cand4, in0=Vv[:, :, 0, :].unsqueeze(3).to_broadcast([128, 8, 16, 16]), in1=Vv[:, :, 1, :].unsqueeze(2).to_broadcast([128, 8, 16, 16]), op=ALU.add), V["a"] + V["b"], [cand])
            for hh in range(2):
                hs_ = range(hh * 4, hh * 4 + 4)
                for h in hs_:
                    S.op("dve", lambda e, h=h, Tt=Tt: e.max(out=Tt["a"][h][:], in_=cand[:, h, :]), [cand], [Tt["a"][h]])
                for h in hs_:
                    S.op("dve", lambda e, h=h, Tt=Tt: e.match_replace(out=tcs[h][:], in_to_replace=Tt["a"][h][:], in_values=cand[:, h, :], imm_value=NEG), [cand, Tt["a"][h]], [tcs[h]])
                for h in hs_:
                    S.op("dve", lambda e, h=h, Tt=Tt: e.max(out=Tt["b"][h][:], in_=tcs[h][:]), [tcs[h]], [Tt["b"][h]])

        def tail(tl):
            V = V16t[tl]; Tt = T16t[tl]; Vv = V["v"]; T16 = Tt["t"]; sm = smalls[tl]
            OFF, OFFv, EX, Z, thrE = sm["OFF"], sm["OFFv"], sm["EX"], sm["Z"], sm["thrE"]
            rdV = V["a"] + V["b"]; rdT = Tt["a"] + Tt["b"]
            S.op("dve", lambda e: e.tensor_copy(out=OFFv[:, :, 0], in_=Vv[:, :, 0, 0]), rdV, [OFF])
            S.op("dve", lambda e: e.scalar_tensor_tensor(out=OFFv[:, :, 1], in0=T16[:, :, 15], scalar=-MARGIN, in1=Vv[:, :, 0, 0], op0=ALU.add, op1=ALU.subtract), rdT + rdV, [OFF])
            S.op("dve", lambda e: e.tensor_tensor(out=EX[:], in0=T16[:], in1=T16[:, :, 0:1].to_broadcast([128, 8, 16]), op=ALU.subtract), rdT, [EX])
            S.op("act", lambda e: e.activation(out=EX[:], in_=EX[:], func=AF.Exp), [EX], [EX])
            S.op("dve", lambda e: e.tensor_reduce(out=Z[:], in_=EX[:], axis=AX.X, op=ALU.add), [EX], [Z])
            S.op("dve", lambda e: e.reciprocal(out=Z[:], in_=Z[:]), [Z], [Z])
            S.op("dve", lambda e: e.scalar_tensor_tensor(out=thrE[:], in0=EX[:, :, 15], scalar=float(math.exp(-MARGIN)), in1=Z[:], op0=ALU.mult, op1=ALU.mult), [EX, Z], [thrE])
            S.op("dve", lambda e, tl=tl: e.tensor_tensor(out=DG[tl][:], in0=ident[:].unsqueeze(1).to_broadcast([128, 8, 128]), in1=thrE[:].unsqueeze(2).to_broadcast([128, 8, 128]), op=ALU.mult), [ident, thrE], [DG[tl]])
            S.op("dve", lambda e, tl=tl: e.tensor_tensor(out=SC[:, tl, :, :], in0=SC[:, tl, :, :], in1=OFF[:].unsqueeze(2).to_broadcast([128, 16, 128]), op=ALU.subtract), [SCt[tl], OFF], [SCt[tl]])
            S.op("act", lambda e, tl=tl: e.activation(out=SC[:, tl, :, :], in_=SC[:, tl, :, :], func=AF.Exp), [SCt[tl]], [SCt[tl]])

        batches(0)
        for tl in range(1, 4):
            batches(tl)
            tail(tl - 1)
        tail(3)
        NGRP = 64
        dts = {}; ups = {}; cur = {}

        def load_w(eg):
            dT = dTr.next(); upc = upr.next()
            S.dma(dT[:], dT_v[:, :, eg * 256:(eg + 1) * 256], [], [dT], dT)
            S.dma(upc[:], C["up_bf"][eg * 256:(eg + 1) * 256, :].rearrange("(ec p) d -> p ec d", p=128), [], [upc], upc)
            dts[eg] = dT; ups[eg] = upc

        def stage1(eg, tl, part=None):
            dT = dts[eg]; GL = GLs[eg % 2]
            if part in (None, 0):
                cur["pa"] = par.next()
            pa = cur["pa"]
            kcs = range(16) if part is None else range(part * 8, part * 8 + 8)
            for kc in kcs:
                S.op("pe", lambda e, pa=pa, kc=kc, tl=tl, dT=dT: e.matmul(pa[:, 0:256], lhsT=h2Tb[:, kc, tl * 128:(tl + 1) * 128], rhs=dT[:, kc, :], start=(kc == 0), stop=(kc == 15)), [h2Tb, dT], [pa])
            if part in (None, 1):
                S.op("act", lambda e, pa=pa, tl=tl, GL=GL: e.activation(out=GL[:, tl, :], in_=pa[:, 0:256], func=AF.Gelu), [pa], [GL])

        def emask_a(k):
            eg, tl = divmod(k, 4)
            Ep = Eps[k % 2]
            S.op("pool", lambda e, tl=tl, eg=eg, Ep=Ep: e.tensor_tensor(out=Ep[:], in0=SCv[:, tl, :, 0, eg * 2:(eg + 1) * 2].unsqueeze(3).to_broadcast([128, 8, 2, 128]), in1=SCv[:, tl, :, 1, :].unsqueeze(2).to_broadcast([128, 8, 2, 128]), op=ALU.mult), [SCt[tl]], [Ep])
            if k % 3 == 2:
                S.op("act", lambda e, Ep=Ep: e.activation(out=Rp0[:].rearrange("p h e -> p (h e)"), in_=Ep[:].rearrange("p h a b -> p (h a b)"), func=AF.Relu, bias=neg1[:]), [Ep, neg1], [Rp0])

        def emask_b(k):
            Ep = Eps[k % 2]
            if k % 3 != 2:
                S.op("dve", lambda e, Ep=Ep: e.scalar_tensor_tensor(out=Mp0[:].rearrange("p h e -> p (h e)"), in0=Ep[:].rearrange("p h a b -> p (h a b)"), scalar=1.0, in1=Ep[:].rearrange("p h a b -> p (h a b)"), op0=ALU.is_ge, op1=ALU.mult), [Ep], [Mp0])

        def emask_c(k):
            if k % 3 == 2:
                S.op("act", lambda e: e.activation(out=Mk0[:], in_=Rp0[:], func=AF.Sign), [Rp0], [Mk0])

        load_w(0)
        for tl in range(4):
            stage1(0, tl)
        emask_a(0); emask_b(0); emask_c(0)
        for k in range(NGRP * 4):
            eg, tl = divmod(k, 4)
            if tl == 0 and eg + 1 < NGRP:
                load_w(eg + 1)
            GL = GLs[eg % 2]; upc = ups[eg]
            Wt = Wtr.next(); WT = WTr.next()
            more = k + 1 < NGRP * 4
            if more:
                emask_a(k + 1)
            if k % 3 != 2:
                for h in range(8):
                    S.op("pe", lambda e, tl=tl, h=h: e.matmul(pg[:, 0:256], lhsT=DG[tl][:, h, :], rhs=Mp0[:, h, :], start=(h == 0), stop=(h == 7)), [DG[tl], Mp0], [pg])
            else:
                for h in range(8):
                    S.op("pe", lambda e, tl=tl, h=h: e.matmul(pg[:, 0:256], lhsT=DG[tl][:, h, :], rhs=Rp0[:, h, :], start=(h == 0), stop=False), [DG[tl], Rp0], [pg])
                for h in range(8):
                    S.op("pe", lambda e, tl=tl, h=h: e.matmul(pg[:, 0:256], lhsT=DG[tl][:, h, :], rhs=Mk0[:, h, :], start=False, stop=(h == 7)), [DG[tl], Mk0], [pg])
            S.op("dve", lambda e, tl=tl, Wt=Wt, GL=GL: e.tensor_tensor(out=Wt[:], in0=pg[:, 0:256], in1=GL[:, tl, :], op=ALU.mult), [pg, GL], [Wt])
            if more:
                emask_b(k + 1)
            if eg + 1 < NGRP:
                stage1(eg + 1, tl, 0)
            for ec in range(2):
                S.op("pe", lambda e, Wt=Wt, ec=ec: e.transpose(out=psT[:, ec * 128:(ec + 1) * 128], in_=Wt[:, ec * 128:(ec + 1) * 128], identity=ident[:]), [Wt, ident], [psT])
            S.op("act", lambda e, WT=WT: e.activation(out=WT[:], in_=psT[:, 0:256], func=AF.Copy), [psT], [WT])
            if more:
                emask_c(k + 1)
            if eg + 1 < NGRP:
                stage1(eg + 1, tl, 1)
            if "dbg" in C and k == C["dbg"]["k"]:
                dd = C["dbg"]
                S.dma(dd["Ep"][:], Eps[k % 2][:].rearrange("p h a b -> p (h a b)"), [Eps[k % 2]], [], Eps[k % 2], queue="act")
                S.dma(dd["Mp"][:], Mp0[:].rearrange("p h e -> p (h e)"), [Mp0], [], Mp0, queue="act")
                S.dma(dd["Wt"][:], Wt[:], [Wt], [], Wt, queue="act")
                S.dma(dd["WT"][:], WT[:], [WT], [], WT, queue="act")
                S.dma(dd["GL"][:], GL[:].rearrange("p a b -> p (a b)"), [GL], [], GL, queue="act")
                S.dma(dd["SC"][:], SC[:].rearrange("p a b c -> p (a b c)"), SCt, [], SC, queue="act")
                S.dma(dd["DG"][:], DG[tl][:].rearrange("p a b -> p (a b)"), [DG[tl]], [], DG[tl], queue="act")
            for half in range(2):
                pbh = psBh[half]
                for dgp in range(2 * half, 2 * half + 2):
                    for ec in range(2):
                        S.op("pe", lambda e, WT=WT, dgp=dgp, ec=ec, upc=upc, pbh=pbh: e.matmul(pbh[:, (dgp % 2) * 512:(dgp % 2 + 1) * 512], lhsT=WT[:, ec * 128:(ec + 1) * 128], rhs=upc[:, ec, dgp * 512:(dgp + 1) * 512], start=(ec == 0), stop=(ec == 1)), [WT, upc], [pbh])
                S.op("dve", lambda e, tl=tl, half=half, pbh=pbh: e.tensor_tensor(out=oaccH[tl][half][:], in0=pbh[:], in1=oaccH[tl][half][:], op=ALU.add), [pbh, oaccH[tl][half]], [oaccH[tl][half]])
        for tl in range(4):
            S.dma(C["out"][c0 + tl * 128:c0 + (tl + 1) * 128, :], oacc[tl][:], [oacc[tl]] + oaccH[tl], [], oacc[tl], queue="act")


def ssm_host(inp):
    f = np.float32
    a_re = inp["ssm_a_re"][0]; a_im = inp["ssm_a_im"][0]; ls = inp["ssm_log_step"][0]
    def lay1(a):
        t = a.reshape(128, 64).T
        return np.ascontiguousarray(np.concatenate([t, t], 0), dtype=f)
    o = {}
    o["s_aim"] = lay1(a_im); o["s_are"] = lay1(a_re)
    o["s_ls"] = np.ascontiguousarray(np.broadcast_to(ls.reshape(1, 128), (128, 128)), dtype=f)
    bre_t = inp["ssm_b_re"][0].reshape(128, 64, 16).transpose(1, 0, 2)
    bim_t = inp["ssm_b_im"][0].reshape(128, 64, 16).transpose(1, 0, 2)
    o["s_x1"] = np.ascontiguousarray(np.concatenate([bre_t, bim_t], 0), dtype=f)
    o["s_x2"] = np.ascontiguousarray(np.concatenate([bim_t, bre_t], 0), dtype=f)
    cre_t = inp["ssm_c_re"][0].reshape(128, 16, 64).transpose(2, 0, 1)
    cim_t = inp["ssm_c_im"][0].reshape(128, 16, 64).transpose(2, 0, 1)
    o["s_ca"] = np.ascontiguousarray(np.concatenate([cre_t, cim_t], 0), dtype=f)
    o["s_cb"] = np.ascontiguousarray(np.concatenate([cim_t, cre_t], 0), dtype=f)
    sg = np.zeros((128, 2), f); sg[:64, 0] = -1; sg[64:, 0] = 1; sg[:64, 1] = 1; sg[64:, 1] = -1
    o["s_sgn"] = sg
    o["ssm_d"] = np.ascontiguousarray(inp["ssm_d"][0].reshape(8, 128).T, dtype=f)
    o["iota"] = np.ascontiguousarray(np.broadcast_to(np.arange(2048, dtype=f), (128, 2048)))
    return o


def attn_host(inp):
    f = np.float32
    slopes = (2.0 ** (-8.0 * (np.arange(16, dtype=np.float64) + 1) / 16))
    tk = np.arange(128)[:, None, None, None]; j = np.arange(3)[None, :, None, None]; tq = np.arange(128)[None, None, None, :]
    dist = np.abs(tq - tk - 128 * (j - 1)).astype(np.float64)
    b = -slopes[None, None, :, None] * dist
    b = np.where(dist <= 128, b, -30000.0)
    o = {"alibi": np.ascontiguousarray(b.reshape(128, 3 * 16 * 128), dtype=f)}
    o["sink_b"] = np.ascontiguousarray(np.broadcast_to(inp["attn_sink"][0][None, :], (128, 16)), dtype=f)
    o["ffn_g"] = np.ascontiguousarray(np.broadcast_to(inp["ffn_norm_g"][0][None, :], (128, 2048)), dtype=f)
    return o


def build_nc(NSEQ=2, SEQ=2048, dbg=False, dbgk=0):
    T = NSEQ * SEQ
    nc = bass.Bass("TRN2", target_bir_lowering=False)
    st = ExitStack()
    with st:
        S = Sched(nc, st)
        S.arena_init(200 * 1024)
        C = {"wq": "pool"}
        EI = "ExternalInput"
        C["x"] = S.dram("x", [T, D], F32, EI)
        for nm, sh in [("w_in", [D, 8192]), ("mix_g", [128, D]), ("qk_g", [128, 2]), ("ident", [128, 128]),
                       ("s_aim", [128, 128]), ("s_are", [128, 128]), ("s_ls", [128, 128]), ("s_x1", [128, 128, 16]), ("s_x2", [128, 128, 16]),
                       ("s_ca", [128, 128, 16]), ("s_cb", [128, 128, 16]), ("s_sgn", [128, 2]), ("ssm_d", [128, 8]), ("iota", [128, 2048]),
                       ("glu_a", [1024, 2048]), ("glu_b", [1024, 2048]), ("alibi", [128, 3 * 16 * 128]), ("sink_b", [128, 16]), ("ffn_g", [128, 2048]),
                       ("w_o", [2048, 2048]), ("w_out", [2048, 2048]), ("peer_wq", [2048, 2048]), ("subk", [128, 16, 128]),
                       ("peer_downT", [2048, 16384]), ("peer_up", [16384, 2048])]:
            C[nm] = S.dram(nm, sh, F32, EI)
        k = "Internal"
        C["qT"] = S.dram("qT", [16, 128, T], BF16, k); C["kT"] = S.dram("kT", [4, 128, T], BF16, k)
        C["v"] = S.dram("v", [T, 512], BF16, k); C["uT"] = S.dram("uT", [8, 128, T], BF16, k)
        C["gaT"] = S.dram("gaT", [16, 128, T], F32, k); C["gbT"] = S.dram("gbT", [16, 128, T], F32, k)
        C["prm"] = S.dram("prm", [128, 128, 64], BF16, k)
        C["MB"] = S.dram("MB", [16, 128, T], F32, k)
        C["yact"] = S.dram("yact", [8, 128, T], BF16, k)
        C["x1"] = S.dram("x1", [T, D], F32, k)
        C["h2T"] = S.dram("h2T", [128, 16, T], BF16, k)
        C["w_in_bf"] = S.dram("w_in_bf", [32, 128, 16, 256], BF16, k)
        C["w_o_bf"] = S.dram("w_o_bf", [16, 128, 16, 128], BF16, k)
        C["w_out_bf"] = S.dram("w_out_bf", [8, 128, 16, 256], BF16, k)
        C["glu_a_bf"] = S.dram("glu_a_bf", [16, 128, 8, 128], BF16, k)
        C["glu_b_bf"] = S.dram("glu_b_bf", [16, 128, 8, 128], BF16, k)
        C["wq_bf"] = S.dram("wq_bf", [16, 128, 16, 128], BF16, k)
        C["downT_bf"] = S.dram("downT_bf", [2048, 16384], BF16, k)
        C["up_bf"] = S.dram("up_bf", [16384, 2048], BF16, k)
        C["out"] = S.dram("out", [T, D], F32, "ExternalOutput")
        if dbg:
            EO = "ExternalOutput"
            C["dbg"] = {"k": dbgk, "Ep": S.dram("d_Ep", [128, 2048], F32, EO), "Mp": S.dram("d_Mp", [128, 2048], BF16, EO),
                        "Wt": S.dram("d_Wt", [128, 256], BF16, EO), "WT": S.dram("d_WT", [128, 256], BF16, EO),
                        "GL": S.dram("d_GL", [128, 1024], BF16, EO), "SC": S.dram("d_SC", [128, 8192], F32, EO),
                        "DG": S.dram("d_DG", [128, 1024], BF16, EO)}
        C["psBig"] = S.psum([128, 2048], F32, "psBig")
        C["psS"] = [S.psum([128, 512], F32, f"psS{i}") for i in range(3)]
        C["psF"] = C["psS"] + [Buf(C["psBig"].t[:, i * 512:(i + 1) * 512], f"psBv{i}") for i in range(4)]
        C["psT"] = S.psum([128, 1024], BF16, "psT")
        idf = S.sbuf([128, 128], F32, "idf"); C["ident_bf"] = S.sbuf([128, 128], BF16, "idb")
        C["ident_f"] = idf
        S.dma(idf[:], C["ident"][:], [], [idf], idf)
        S.op("dve", lambda e: e.tensor_copy(out=C["ident_bf"][:], in_=idf[:]), [idf], [C["ident_bf"]])
        phase_P0(S, C)
        S.arena_reset(); phase_A(S, C, T)
        S.arena_reset(); phase_C0(S, C)
        S.arena_reset(); phase_C1(S, C, NSEQ, SEQ)
        S.arena_reset(); phase_C2(S, C, T)
        S.arena_reset(); phase_B(S, C, NSEQ, SEQ)
        S.arena_reset(); phase_D(S, C, T)
        S.barrier()
        for e in S.ENG:
            S.op(e, lambda e: e.nop(), [], [])
        S.emit()
        build_nc.stats = (len(S.ops), S.nsem)
    return nc


def host_inputs(inp):
    f = np.float32
    c = lambda a: np.ascontiguousarray(a, dtype=f)
    o = {}
    o["w_in"] = c(inp["w_in"][0])
    o["mix_g"] = c(np.broadcast_to(inp["mix_norm_g"][0][None, :], (128, D)))
    o["qk_g"] = c(np.stack([inp["q_norm_g"][0], inp["k_norm_g"][0]], 1))
    o["ident"] = np.eye(128, dtype=f)
    o.update(ssm_host(inp))
    o.update(attn_host(inp))
    o["glu_a"] = c(inp["glu_w_a"][0]); o["glu_b"] = c(inp["glu_w_b"][0])
    o["w_o"] = c(inp["w_attn_o"][0]); o["w_out"] = c(inp["w_out"][0])
    o["peer_wq"] = c(inp["peer_w_query"][0])
    k1 = inp["peer_sub_keys_1"][0]; k2 = inp["peer_sub_keys_2"][0]
    sk = np.stack([k1, k2], 1)
    o["subk"] = c(sk.transpose(3, 0, 1, 2).reshape(128, 16, 128))
    o["peer_downT"] = c(inp["peer_down"][0].T)
    o["peer_up"] = c(inp["peer_up"][0])
    return o


_NC_CACHE = {}


def kernel(**inputs):
    x = np.asarray(inputs["x"], dtype=np.float32)
    B, SEQ, _ = x.shape
    ncores = 8
    NSEQ = B // ncores
    key = (NSEQ, SEQ)
    if key not in _NC_CACHE:
        _NC_CACHE[key] = build_nc(NSEQ, SEQ)
    nc = _NC_CACHE[key]
    shared = host_inputs(inputs)
    in_maps = []
    for i in range(ncores):
        m = dict(shared)
        m["x"] = np.ascontiguousarray(x[i * NSEQ:(i + 1) * NSEQ].reshape(NSEQ * SEQ, D))
        in_maps.append(m)
    res = run_bass_kernel_spmd(nc, in_maps, core_ids=list(range(ncores)))
    outs = [np.asarray(r["out"], dtype=np.float32).reshape(NSEQ, SEQ, D) for r in res.results]
    return np.concatenate(outs, axis=0)
```

```python
import math
import numpy as np
import concourse.bass as bass
import concourse.mybir as mybir
from concourse.bass_utils import run_bass_kernel_spmd
from contextlib import ExitStack

F32 = mybir.dt.float32
BF16 = mybir.dt.bfloat16
I32 = mybir.dt.int32
ALU = mybir.AluOpType
AF = mybir.ActivationFunctionType
AX = mybir.AxisListType

SAME_ENGINE_SYNC = True
SEM_LIMIT = 30000


class Buf:
    __slots__ = ("t", "lw", "rd", "name")

    def __init__(self, t, name=""):
        self.t = t
        self.lw = None
        self.rd = {}
        self.name = name

    def __getitem__(self, k):
        return self.t[k]


class Op:
    __slots__ = ("eng", "fn", "deps", "is_dma", "key", "signal", "sem", "count", "phase")

    def __init__(self, eng, fn, deps, is_dma=False, key=None):
        self.eng = eng
        self.fn = fn
        self.deps = deps
        self.is_dma = is_dma
        self.key = key
        self.signal = is_dma
        self.sem = None
        self.count = 0


class Sched:
    ENG = ("pe", "act", "dve", "pool", "sp")

    def __init__(self, nc, stack):
        self.nc = nc
        self.stack = stack
        self.ops = []
        self.streams = {e: [] for e in self.ENG}
        self.key_last = {}
        self.nbuf = 0
        self.barrier_deps = {e: set() for e in self.ENG}
        self.phase = 0

    def sbuf(self, shape, dtype, name=None):
        self.nbuf += 1
        name = name or f"sb{self.nbuf}"
        t = self.stack.enter_context(self.nc.sbuf_tensor(name, list(shape), dtype))
        return Buf(t, name)

    def psum(self, shape, dtype, name=None):
        self.nbuf += 1
        name = name or f"ps{self.nbuf}"
        t = self.stack.enter_context(self.nc.psum_tensor(name, list(shape), dtype))
        return Buf(t, name)

    def dram(self, name, shape, dtype, kind="Internal"):
        t = self.nc.dram_tensor(name, list(shape), dtype, kind=kind)
        return Buf(t.ap(), name)

    def view(self, buf_ap, name=""):
        return Buf(buf_ap, name)

    def _mkdeps(self, eng, reads, writes, key_tag):
        deps = set()
        for b in reads:
            if b.lw is not None:
                deps.add(b.lw)
        for b in writes:
            if b.lw is not None:
                deps.add(b.lw)
            deps.update(b.rd.values())
        if self.barrier_deps[eng]:
            deps.update(self.barrier_deps[eng])
            self.barrier_deps[eng] = set()
        return deps

    def op(self, eng, fn, reads=(), writes=()):
        deps = self._mkdeps(eng, reads, writes, eng)
        oid = len(self.ops)
        self.ops.append(Op(eng, fn, deps))
        self.streams[eng].append(oid)
        for b in writes:
            b.lw = oid
            b.rd = {}
        ws = set(id(b) for b in writes)
        for b in reads:
            if id(b) not in ws:
                b.rd[eng] = oid
        return oid

    def dma(self, out_ap, in_ap, reads, writes, key, queue="sp", **kw):
        deps = self._mkdeps(queue, reads, writes, None)
        kid = id(key)
        if kid in self.key_last:
            deps.add(self.key_last[kid])
        oid = len(self.ops)

        def fn(e, out_ap=out_ap, in_ap=in_ap, kw=kw):
            return e.dma_start(out=out_ap, in_=in_ap, **kw)

        self.ops.append(Op(queue, fn, deps, True, key))
        self.ops[-1].phase = self.phase
        self.streams[queue].append(oid)
        self.key_last[kid] = oid
        for b in writes:
            b.lw = oid
            b.rd = {}
        ws = set(id(b) for b in writes)
        for b in reads:
            if id(b) not in ws:
                b.rd[("dma", kid)] = oid
        return oid

    def barrier(self):
        last = set()
        for e in self.ENG:
            for oid in reversed(self.streams[e]):
                if not self.ops[oid].is_dma:
                    last.add(oid)
                    break
        last.update(self.key_last.values())
        for e in self.ENG:
            self.barrier_deps[e] = set(last)
        self.phase += 1

    def emit(self):
        nc = self.nc
        ops = self.ops
        for op in ops:
            for d in op.deps:
                ops[d].signal = True
        nsem = [0]

        def newsem():
            nsem[0] += 1
            return self.stack.enter_context(nc.semaphore(f"s{nsem[0]}"))

        for eng in self.ENG:
            cur = None
            cnt = 0
            for oid in self.streams[eng]:
                op = ops[oid]
                if op.is_dma or not op.signal:
                    continue
                if cur is None or cnt >= SEM_LIMIT:
                    cur = newsem()
                    cnt = 0
                cnt += 1
                op.sem = cur
                op.count = cnt
        kstate = {}
        freepool = {True: [], False: []}
        kq = {}
        curphase = 0
        for op in ops:
            if not op.is_dma:
                continue
            if op.phase != curphase:
                curphase = op.phase
                for kk, st in kstate.items():
                    if st[1] + 16 <= SEM_LIMIT:
                        freepool[kq[kk]].append(st)
                kstate = {}
            kid = id(op.key)
            st = kstate.get(kid)
            if st is None or st[1] + 16 > SEM_LIMIT:
                sw = (op.eng == "pool")
                st = freepool[sw].pop() if freepool[sw] else [newsem(), 0]
                kstate[kid] = st
                kq[kid] = sw
            st[1] += 16
            op.sem = st[0]
            op.count = st[1]
        self.nsem = nsem[0]

        with nc.Block() as block:
            for eng in self.ENG:
                stream = self.streams[eng]
                if not stream:
                    continue

                def body(e, eng=eng, stream=stream):
                    waited = {}
                    for oid in stream:
                        op = ops[oid]
                        for d in sorted(op.deps):
                            dop = ops[d]
                            if dop.eng == eng and not dop.is_dma:
                                if eng == "pe" or not SAME_ENGINE_SYNC:
                                    continue
                            sid = id(dop.sem)
                            if waited.get(sid, 0) >= dop.count:
                                continue
                            e.wait_ge(dop.sem, dop.count)
                            waited[sid] = dop.count
                        ins = op.fn(e)
                        if op.signal:
                            ins.then_inc(op.sem, 16 if op.is_dma else 1)

                {"pe": block.tensor, "act": block.scalar, "dve": block.vector,
                 "pool": block.gpsimd, "sp": block.sync}[eng](body)


def _arena_init(self, nbytes):
    self.arena = self.stack.enter_context(self.nc.sbuf_tensor("arena", [128, nbytes // 4], F32))
    self.aoff = 0
    self.amax = nbytes // 4


def _arena_reset(self):
    self.barrier()
    self.aoff = 0


def _alloc(self, shape, dtype, name=""):
    esz = 2 if dtype == BF16 else 4
    nfree = 1
    for s in shape[1:]:
        nfree *= s
    nwords = (nfree * esz + 3) // 4
    nwords_al = (nwords + 15) // 16 * 16
    off = self.aoff
    assert off + nwords_al <= self.amax, f"arena overflow {name} {off} {nwords_al} {self.amax}"
    self.aoff += nwords_al
    ap = self.arena[0:shape[0], off:off + nwords]
    if dtype != F32:
        ap = ap.bitcast(dtype)
        if ap.shape[1] != nfree:
            ap = ap[:, 0:nfree]
    if len(shape) == 3:
        ap = ap.rearrange("p (a b) -> p a b", b=shape[2])
    elif len(shape) == 4:
        ap = ap.rearrange("p (a b c) -> p a b c", b=shape[2], c=shape[3])
    return Buf(ap, name)


Sched.arena_init = _arena_init
Sched.arena_reset = _arena_reset
Sched.alloc = _alloc


class Ring:
    def __init__(self, bufs):
        self.b = bufs
        self.i = 0

    def next(self):
        b = self.b[self.i % len(self.b)]
        self.i += 1
        return b


D = 2048
EPS = 1e-6

class _RingOld:
    def __init__(self, bufs): self.b = bufs; self.i = 0
    def next(self):
        b = self.b[self.i % len(self.b)]; self.i += 1; return b

def phase_A(S, C, T):
    nc = S.nc
    NB = T // 512
    x, w_in = C["x"], C["w_in"]
    psF, psT = C["psF"], C["psT"]
    ident = C["ident_bf"]
    gmix = S.alloc([128, D], F32, "gmix")
    S.dma(gmix[:], C["mix_g"][:], [], [gmix], gmix)
    gqk = S.alloc([128, 2], F32, "gqk")
    S.dma(gqk[:], C["qk_g"][:], [], [gqk], gqk)
    S.op("dve", lambda e: e.tensor_scalar(out=gqk[:, 0:1], in0=gqk[:, 0:1], scalar1=float(128 ** -0.5), scalar2=None, op0=ALU.mult), [gqk], [gqk])
    epsD = S.alloc([128, 1], F32, "epsD")
    S.op("dve", lambda e: e.memset(epsD[:], EPS), [], [epsD])
    ones_f = S.alloc([128, 128], F32, "ones_f")
    S.op("dve", lambda e: e.memset(ones_f[:], 1.0 / 128.0), [], [ones_f])
    xring = Ring([S.alloc([128, D], F32, f"xt{i}") for i in range(2)])
    hring = Ring([S.alloc([128, D], BF16, f"h{i}") for i in range(2)])
    junk = S.alloc([128, D], BF16, "junkA")
    hTring = Ring([S.alloc([128, 16, 512], BF16, f"hT{i}") for i in range(2)])
    wring = Ring([S.alloc([128, 16, 256], BF16, f"w{i}") for i in range(3)])
    sring = Ring([S.alloc([128, 1], F32, f"ss{i}") for i in range(4)])
    sqring = Ring([S.alloc([128, 512], F32, f"sq{i}") for i in range(2)])
    rsring = Ring([S.alloc([128, 512], F32, f"rs{i}") for i in range(2)])
    obring = Ring([S.alloc([128, 512], BF16, f"ob{i}") for i in range(4)])
    ofring = Ring([S.alloc([128, 512], F32, f"of{i}") for i in range(4)])
    pring = Ring(psF[0:5])
    p1ring = Ring(psF[5:7])
    w_v = w_in.t.rearrange("(kc p) f -> p kc f", p=128)
    pgen = peer_conv_gen(S, C)
    nconv = 128
    for tb in range(NB):
        hT = hTring.next()
        for tl in range(4):
            r0 = tb * 512 + tl * 128
            xt = xring.next(); h = hring.next(); ss = sring.next()
            S.dma(xt[:], x[r0:r0 + 128, :], [], [xt], xt)
            S.op("act", lambda e, xt=xt, ss=ss: e.activation(out=junk[:], in_=xt[:], func=AF.Square, accum_out=ss[:]), [xt], [junk, ss])
            S.op("act", lambda e, ss=ss: e.activation(out=ss[:], in_=ss[:], func=AF.Sqrt, scale=1.0 / D, bias=epsD[:]), [ss, epsD], [ss])
            S.op("dve", lambda e, ss=ss: e.reciprocal(out=ss[:], in_=ss[:]), [ss], [ss])
            S.op("dve", lambda e, xt=xt, ss=ss, h=h: e.scalar_tensor_tensor(out=h[:], in0=xt[:], scalar=ss[:, 0:1], in1=gmix[:], op0=ALU.mult, op1=ALU.mult), [xt, ss, gmix], [h])
            for half in range(2):
                for j in range(8):
                    kc = half * 8 + j
                    S.op("pe", lambda e, h=h, kc=kc, j=j: e.transpose(out=psT[:, j * 128:(j + 1) * 128], in_=h[:, kc * 128:(kc + 1) * 128], identity=ident[:]), [h, ident], [psT])
                S.op("act", lambda e, hT=hT, half=half, tl=tl: e.activation(out=hT[:, half * 8:half * 8 + 8, tl * 128:(tl + 1) * 128], in_=psT[:].rearrange("p (a b) -> p a b", b=128), func=AF.Copy), [psT], [hT])
        c0 = tb * 512
        for fg in range(32):
            if (fg % 2 == 0) and True:
                for _ in range(max(1, nconv // (NB * 16))):
                    next(pgen, None)
            wt = wring.next()
            S.dma(wt[:], C["w_in_bf"][fg], [], [wt], wt)
            if fg in (10, 11):
                for tl in range(4):
                    pv = pring.next()
                    for kc in range(16):
                        S.op("pe", lambda e, pv=pv, hT=hT, wt=wt, kc=kc, tl=tl: e.matmul(pv[:, 0:256], lhsT=hT[:, kc, tl * 128:(tl + 1) * 128], rhs=wt[:, kc, :], start=(kc == 0), stop=(kc == 15)), [hT, wt], [pv])
                    ob = obring.next()
                    S.op("act", lambda e, pv=pv, ob=ob: e.activation(out=ob[:, 0:256], in_=pv[:, 0:256], func=AF.Copy), [pv], [ob])
                    r0 = c0 + tl * 128
                    S.dma(C["v"][r0:r0 + 128, (fg - 10) * 256:(fg - 9) * 256], ob[:, 0:256], [ob], [], ob, queue=C.get("stq", "act"))
                continue
            for c2 in range(2):
                fc = fg * 2 + c2
                pq = pring.next()
                for kc in range(16):
                    S.op("pe", lambda e, pq=pq, hT=hT, wt=wt, kc=kc, c2=c2: e.matmul(pq[:], lhsT=wt[:, kc, c2 * 128:(c2 + 1) * 128], rhs=hT[:, kc, :], start=(kc == 0), stop=(kc == 15)), [hT, wt], [pq])
                if fc < 20:
                    sq = sqring.next(); rs = rsring.next(); p1 = p1ring.next(); ob = obring.next()
                    S.op("act", lambda e, pq=pq, sq=sq: e.activation(out=sq[:], in_=pq[:], func=AF.Square), [pq], [sq])
                    S.op("pe", lambda e, p1=p1, sq=sq: e.matmul(p1[:], lhsT=ones_f[:], rhs=sq[:], start=True, stop=True), [ones_f, sq], [p1])
                    S.op("act", lambda e, p1=p1, rs=rs: e.activation(out=rs[:], in_=p1[:], func=AF.Sqrt, bias=epsD[:]), [p1, epsD], [rs])
                    S.op("dve", lambda e, rs=rs: e.reciprocal(out=rs[:], in_=rs[:]), [rs], [rs])
                    gi = 0 if fc < 16 else 1
                    S.op("dve", lambda e, pq=pq, rs=rs, ob=ob, gi=gi: e.scalar_tensor_tensor(out=ob[:], in0=pq[:], scalar=gqk[:, gi:gi + 1], in1=rs[:], op0=ALU.mult, op1=ALU.mult), [pq, rs, gqk], [ob])
                    dst = C["qT"][fc, :, c0:c0 + 512] if fc < 16 else C["kT"][fc - 16, :, c0:c0 + 512]
                    S.dma(dst, ob[:], [ob], [], ob, queue=C.get("stq", "act"))
                elif fc < 32:
                    ob = obring.next()
                    S.op("act", lambda e, pq=pq, ob=ob: e.activation(out=ob[:], in_=pq[:], func=AF.Copy), [pq], [ob])
                    S.dma(C["uT"][fc - 24, :, c0:c0 + 512], ob[:], [ob], [], ob, queue=C.get("stq", "act"))
                else:
                    of = ofring.next()
                    S.op("act", lambda e, pq=pq, of=of: e.activation(out=of[:], in_=pq[:], func=AF.Sigmoid), [pq], [of])
                    dst = C["gaT"][fc - 32, :, c0:c0 + 512] if fc < 48 else C["gbT"][fc - 48, :, c0:c0 + 512]
                    S.dma(dst, of[:], [of], [], of, queue=C.get("stq", "act"))
    for _ in pgen:
        pass


PI = math.pi
SINSC = 1.0 - 2e-6


def bc3(ap2, n):
    return ap2.unsqueeze(2).to_broadcast([ap2.shape[0], ap2.shape[1], n])


def sincos(S, A, TH, shape, nm):
    KI = A(shape, I32, nm + "ki"); KF = A(shape, F32, nm + "kf"); ARG = A(shape, F32, nm + "arg")
    SN = A(shape, F32, nm + "sn"); CS = A(shape, F32, nm + "cs")
    S.op("dve", lambda e: e.tensor_scalar(out=KI[:], in0=TH[:], scalar1=1.0 / (2 * PI), scalar2=None, op0=ALU.mult), [TH], [KI])
    S.op("dve", lambda e: e.tensor_copy(out=KF[:], in_=KI[:]), [KI], [KF])
    S.op("dve", lambda e: e.scalar_tensor_tensor(out=ARG[:], in0=KF[:], scalar=-2 * PI, in1=TH[:], op0=ALU.mult, op1=ALU.add), [KF, TH], [ARG])
    S.op("act", lambda e: e.activation(out=SN[:], in_=ARG[:], func=AF.Sin, scale=SINSC), [ARG], [SN])
    S.op("dve", lambda e: e.tensor_scalar(out=KF[:], in0=ARG[:], scalar1=PI / 2, scalar2=-2 * PI, op0=ALU.is_gt, op1=ALU.mult), [ARG], [KF])
    S.op("dve", lambda e: e.scalar_tensor_tensor(out=ARG[:], in0=ARG[:], scalar=PI / 2, in1=KF[:], op0=ALU.add, op1=ALU.add), [ARG, KF], [ARG])
    S.op("act", lambda e: e.activation(out=CS[:], in_=ARG[:], func=AF.Sin, scale=SINSC), [ARG], [CS])
    return SN, CS


def phase_C0(S, C):
    A = S.alloc
    sh = [128, 128]
    TH = C["TH"] = S.sbuf(sh, F32, "TH"); R = C["R"] = S.sbuf(sh, F32, "Rdec"); TH2 = C["TH2"] = S.sbuf(sh, F32, "TH2")
    aim = A(sh, F32); are = A(sh, F32); ls = A(sh, F32); sgn = A([128, 2], F32)
    S.dma(aim[:], C["s_aim"][:], [], [aim], aim)
    S.dma(are[:], C["s_are"][:], [], [are], are)
    S.dma(ls[:], C["s_ls"][:], [], [ls], ls)
    S.dma(sgn[:], C["s_sgn"][:], [], [sgn], sgn)
    S.op("act", lambda e: e.activation(out=ls[:], in_=ls[:], func=AF.Exp), [ls], [ls])
    S.op("dve", lambda e: e.tensor_tensor(out=TH[:], in0=aim[:], in1=ls[:], op=ALU.mult), [aim, ls], [TH])
    S.op("dve", lambda e: e.tensor_scalar(out=TH2[:], in0=TH[:], scalar1=1.0 / (2 * PI), scalar2=None, op0=ALU.mult), [TH], [TH2])
    AR = A(sh, F32)
    S.op("dve", lambda e: e.tensor_tensor(out=AR[:], in0=are[:], in1=ls[:], op=ALU.mult), [are, ls], [AR])
    S.op("act", lambda e: e.activation(out=R[:], in_=AR[:], func=AF.Exp), [AR], [R])
    SN, CS = sincos(S, A, TH, sh, "c0")
    nr = A(sh, F32); ni = A(sh, F32); t1 = A(sh, F32); t2 = A(sh, F32); cr = A(sh, F32); ci = A(sh, F32)
    tt = lambda o, a, b, op, rd: S.op("dve", lambda e: e.tensor_tensor(out=o[:], in0=a[:], in1=b[:], op=op), rd, [o])
    tt(nr, R, CS, ALU.mult, [R, CS])
    S.op("dve", lambda e: e.tensor_scalar(out=nr[:], in0=nr[:], scalar1=-1.0, scalar2=None, op0=ALU.add), [nr], [nr])
    tt(ni, R, SN, ALU.mult, [R, SN])
    tt(t1, are, are, ALU.mult, [are])
    tt(t2, aim, aim, ALU.mult, [aim])
    tt(t1, t1, t2, ALU.add, [t1, t2])
    S.op("dve", lambda e: e.reciprocal(out=t1[:], in_=t1[:]), [t1], [t1])
    tt(cr, nr, are, ALU.mult, [nr, are])
    tt(t2, ni, aim, ALU.mult, [ni, aim])
    tt(cr, cr, t2, ALU.add, [cr, t2])
    tt(cr, cr, t1, ALU.mult, [cr, t1])
    tt(ci, ni, are, ALU.mult, [ni, are])
    tt(t2, nr, aim, ALU.mult, [nr, aim])
    tt(ci, ci, t2, ALU.subtract, [ci, t2])
    tt(ci, ci, t1, ALU.mult, [ci, t1])
    cis1 = A(sh, F32); crs2 = A(sh, F32)
    S.op("dve", lambda e: e.tensor_scalar(out=cis1[:], in0=ci[:], scalar1=sgn[:, 0:1], scalar2=None, op0=ALU.mult), [ci, sgn], [cis1])
    S.op("dve", lambda e: e.tensor_scalar(out=crs2[:], in0=cr[:], scalar1=sgn[:, 0:1], scalar2=-1.0, op0=ALU.mult, op1=ALU.mult), [cr, sgn], [crs2])
    sh3 = [128, 128, 16]
    X1 = A(sh3, F32); X2 = A(sh3, F32); LA = A(sh3, F32); LB = A(sh3, F32)
    PRM = A([128, 128, 4, 16], BF16)
    S.dma(X1[:], C["s_x1"][:], [], [X1], X1)
    S.dma(X2[:], C["s_x2"][:], [], [X2], X2)
    ttb = lambda o, a, b2, rd: S.op("dve", lambda e: e.tensor_tensor(out=o[:], in0=a[:], in1=bc3(b2[:], 16), op=ALU.mult), rd, [o])
    ttb(LA, X1, cr, [X1, cr]); ttb(LB, X2, cis1, [X2, cis1])
    S.op("dve", lambda e: e.tensor_tensor(out=PRM[:, :, 0, :], in0=LA[:], in1=LB[:], op=ALU.add), [LA, LB], [PRM])
    ttb(LA, X2, crs2, [X2, crs2]); ttb(LB, X1, ci, [X1, ci])
    S.op("dve", lambda e: e.tensor_tensor(out=PRM[:, :, 1, :], in0=LA[:], in1=LB[:], op=ALU.add), [LA, LB], [PRM])
    S.dma(X1[:], C["s_ca"][:], [], [X1], X1)
    S.dma(X2[:], C["s_cb"][:], [], [X2], X2)
    S.op("dve", lambda e: e.tensor_scalar(out=PRM[:, :, 2, :], in0=X1[:], scalar1=sgn[:, 1:2], scalar2=None, op0=ALU.mult), [X1, sgn], [PRM])
    S.op("dve", lambda e: e.tensor_scalar(out=PRM[:, :, 3, :], in0=X2[:], scalar1=-1.0, scalar2=None, op0=ALU.mult), [X2], [PRM])
    S.dma(C["prm"][:], PRM[:].rearrange("p a b c -> p a (b c)"), [PRM], [], PRM)


def phase_C1(S, C, NSEQ, SEQ):
    A = S.alloc
    T = NSEQ * SEQ
    NP = SEQ // 512
    psF, psT, ident = C["psF"], C["psT"], C["ident_bf"]
    TH, R, TH2 = C["TH"], C["R"], C["TH2"]
    iota = A([128, SEQ], F32, "iota")
    S.dma(iota[:], C["iota"][:, 0:SEQ], [], [iota], iota)
    dcol = A([128, 8], F32, "dcol")
    S.dma(dcol[:], C["ssm_d"][:], [], [dcol], dcol)
    hpi = A([128, 1], F32, "hpi")
    S.op("dve", lambda e: e.memset(hpi[:], PI / 2), [], [hpi])
    HS = min(1024, SEQ)
    NH = SEQ // HS
    COSr = Ring([A([128, SEQ], F32, f"COS{i}") for i in range(3)])
    SINr = Ring([A([128, SEQ], F32, f"SIN{i}") for i in range(3)])
    magic = A([128, 1], F32, "magic"); nmagic = A([128, 1], F32, "nmagic")
    S.op("dve", lambda e: e.memset(magic[:], 12582912.0), [], [magic])
    S.op("dve", lambda e: e.memset(nmagic[:], -12582912.0), [], [nmagic])
    KFs = [A([128, HS], F32, "KF0")] * NH
    ARGs = [A([128, HS], F32, "ARG0")] * NH
    ABs = [A([128, HS], F32, "AB0")] * NH
    Wb = [A([128, SEQ], F32, f"W{i}") for i in range(2)]
    Vb = [A([128, SEQ], F32, f"V{i}") for i in range(2)]
    PCr = Ring([A([128, SEQ], BF16, f"PC{i}") for i in range(2 * NSEQ)])
    PSr = Ring([A([128, SEQ], BF16, f"PSb{i}") for i in range(2 * NSEQ)])
    yacc = A([128, T], F32, "yacc")
    yst = A([128, T], BF16, "yst")
    uch = A([128, T], BF16, "uch")
    ugr = [A([128, T], BF16, f"ug{i}") for i in range(2)]
    for ug in ugr:
        S.op("pool", lambda e, ug=ug: e.memset(ug[:], 0.0), [], [ug])
    ugring = Ring(ugr)
    Lr = [A([128, 2, 128], BF16, f"L{i}") for i in range(2)]
    for L in Lr:
        S.op("pool", lambda e, L=L: e.memset(L[:], 0.0), [], [L])
    Lring = Ring(Lr)
    CABring = Ring([A([128, 2, 128], BF16, f"CAB{i}") for i in range(2)])
    prmring = Ring([A([128, 64], BF16, f"prm{i}") for i in range(3)])
    tAr = Ring([A([128, 512], F32, f"tA{i}") for i in range(2)])
    tBr = Ring([A([128, 512], F32, f"tB{i}") for i in range(2)])
    bur = Ring(psF[0:4])
    pyr = Ring(psF[4:7])
    iters = [(cc, gi, d) for cc in range(8) for gi in range(8) for d in range(2)]
    state = {}

    tabs = {}

    def tables(it):
        cc, gi, d = it
        dg = d * 64 + cc * 8 + gi
        COS = COSr.next(); SIN = SINr.next()
        tabs[it] = (COS, SIN)
        for hs in range(NH):
            sl = slice(hs * HS, (hs + 1) * HS)
            S.op("act", lambda e, sl=sl, dg=dg, hs=hs: e.activation(out=KFs[hs][:], in_=iota[:, sl], func=AF.Identity, scale=TH2[:, dg:dg + 1], bias=magic[:]), [iota, TH2, magic], [KFs[hs]])
            S.op("act", lambda e, hs=hs: e.activation(out=KFs[hs][:], in_=KFs[hs][:], func=AF.Identity, bias=nmagic[:]), [KFs[hs], nmagic], [KFs[hs]])
            S.op("dve", lambda e, sl=sl, dg=dg, hs=hs: e.scalar_tensor_tensor(out=ARGs[hs][:], in0=iota[:, sl], scalar=TH2[:, dg:dg + 1], in1=KFs[hs][:], op0=ALU.mult, op1=ALU.subtract), [iota, TH2, KFs[hs]], [ARGs[hs]])
            S.op("act", lambda e, sl=sl, hs=hs, SIN=SIN: e.activation(out=SIN[:, sl], in_=ARGs[hs][:], func=AF.Sin, scale=2 * PI * SINSC), [ARGs[hs]], [SIN])
            S.op("act", lambda e, hs=hs: e.activation(out=ABs[hs][:], in_=ARGs[hs][:], func=AF.Abs), [ARGs[hs]], [ABs[hs]])
            S.op("act", lambda e, sl=sl, hs=hs, COS=COS: e.activation(out=COS[:, sl], in_=ABs[hs][:], func=AF.Sin, scale=-2 * PI * SINSC, bias=hpi[:]), [ABs[hs], hpi], [COS])


    def head(it):
        cc, gi, d = it
        g = cc * 8 + gi
        dg = d * 64 + g
        if gi == 0 and d == 0:
            S.dma(uch[:], C["uT"][cc, :, :], [], [uch], uch)
        if d == 0:
            ug = ugring.next()
            S.dma(ug[0:16, :], C["uT"][cc, gi * 16:(gi + 1) * 16, :], [], [ug], ug)
            state["ug"] = ug
        ug = state["ug"]
        prm = prmring.next(); L = Lring.next(); CAB = CABring.next()
        S.dma(prm[:], C["prm"][:, dg, :], [], [prm], prm)
        for j in range(2):
            S.op("pe", lambda e, prm=prm, j=j: e.transpose(out=psT[0:16, j * 128:(j + 1) * 128], in_=prm[:, j * 16:(j + 1) * 16], identity=ident[:]), [prm, ident], [psT])
        S.op("act", lambda e, L=L: e.activation(out=L[0:16, :, :], in_=psT[0:16, 0:256].rearrange("p (a b) -> p a b", b=128), func=AF.Copy), [psT], [L])
        S.op("pool", lambda e, CAB=CAB: e.memset(CAB[:], 0.0), [], [CAB])
        S.op("pool", lambda e, CAB=CAB, prm=prm, gi=gi: e.tensor_copy(out=CAB[:, :, gi * 16:(gi + 1) * 16], in_=prm[:, 32:64].rearrange("p (a b) -> p a b", b=16)), [prm], [CAB])
        COS, SIN = tabs.pop(it)
        def tsl(ap, p, d=d):
            if d == 0:
                return ap[:, p * 512:(p + 1) * 512]
            return ap[:, SEQ - (p + 1) * 512:SEQ - p * 512][:, ::-1]
        for b in range(NSEQ):
            W = Wb[b]
            for p in range(NP):
                c0 = b * SEQ + p * 512
                b1 = bur.next(); b2 = bur.next(); tA = tAr.next(); tB = tBr.next()
                S.op("pe", lambda e, b1=b1, L=L, ug=ug, c0=c0: e.matmul(b1[:], lhsT=L[:, 0, :], rhs=ug[:, c0:c0 + 512], start=True, stop=True), [L, ug], [b1])
                S.op("pe", lambda e, b2=b2, L=L, ug=ug, c0=c0: e.matmul(b2[:], lhsT=L[:, 1, :], rhs=ug[:, c0:c0 + 512], start=True, stop=True), [L, ug], [b2])
                S.op("dve", lambda e, tA=tA, b1=b1, p=p, tsl=tsl, COS=COS: e.tensor_tensor(out=tA[:], in0=b1[:], in1=tsl(COS, p), op=ALU.mult), [b1, COS], [tA])
                S.op("dve", lambda e, tB=tB, b2=b2, p=p, tsl=tsl, SIN=SIN: e.tensor_tensor(out=tB[:], in0=b2[:], in1=tsl(SIN, p), op=ALU.mult), [b2, SIN], [tB])
                S.op("pool", lambda e, tA=tA, tB=tB, p=p, W=W: e.tensor_tensor(out=W[:, p * 512:(p + 1) * 512], in0=tA[:], in1=tB[:], op=ALU.add), [tA, tB], [W])
        pcs = []
        for b in range(NSEQ):
            W = Wb[b]; V = Vb[b]; PC = PCr.next(); PS = PSr.next()
            if d == 0:
                S.op("dve", lambda e, dg=dg, W=W, V=V: e.tensor_tensor_scan(out=V[:], data0=R[:, dg:dg + 1].to_broadcast([128, SEQ]), data1=W[:], initial=0.0, op0=ALU.mult, op1=ALU.add), [R, W], [V])
                S.op("pool", lambda e, V=V, PC=PC, COS=COS: e.tensor_tensor(out=PC[:], in0=V[:], in1=COS[:], op=ALU.mult), [V, COS], [PC])
                S.op("pool", lambda e, V=V, PS=PS, SIN=SIN: e.tensor_tensor(out=PS[:], in0=V[:], in1=SIN[:], op=ALU.mult), [V, SIN], [PS])
            else:
                S.op("dve", lambda e, dg=dg, W=W, V=V: e.tensor_tensor_scan(out=V[:, ::-1], data0=R[:, dg:dg + 1].to_broadcast([128, SEQ]), data1=W[:, ::-1], initial=0.0, op0=ALU.mult, op1=ALU.add), [R, W], [V])
                S.op("pool", lambda e, V=V, PC=PC, COS=COS: e.tensor_tensor(out=PC[:], in0=V[:], in1=COS[:, ::-1], op=ALU.mult), [V, COS], [PC])
                S.op("pool", lambda e, V=V, PS=PS, SIN=SIN: e.tensor_tensor(out=PS[:], in0=V[:], in1=SIN[:, ::-1], op=ALU.mult), [V, SIN], [PS])
            pcs.append((PC, PS))
        return (CAB, pcs)

    def tail(it, hd):
        cc, gi, d = it
        CAB, pcs = hd
        if gi == 0 and d == 0:
            S.op("dve", lambda e, cc=cc: e.tensor_scalar(out=yacc[:], in0=uch[:], scalar1=dcol[:, cc:cc + 1], scalar2=None, op0=ALU.mult), [uch, dcol], [yacc])
        for b in range(NSEQ):
            PC, PS = pcs[b]
            for p in range(NP):
                c0 = b * SEQ + p * 512
                py = pyr.next()
                S.op("pe", lambda e, py=py, CAB=CAB, p=p, PC=PC: e.matmul(py[:], lhsT=CAB[:, 0, :], rhs=PC[:, p * 512:(p + 1) * 512], start=True, stop=False), [CAB, PC], [py])
                S.op("pe", lambda e, py=py, CAB=CAB, p=p, PS=PS: e.matmul(py[:], lhsT=CAB[:, 1, :], rhs=PS[:, p * 512:(p + 1) * 512], start=False, stop=True), [CAB, PS], [py])
                S.op("dve", lambda e, py=py, c0=c0: e.tensor_tensor(out=yacc[:, c0:c0 + 512], in0=py[:], in1=yacc[:, c0:c0 + 512], op=ALU.add), [py, yacc], [yacc])
        if gi == 7 and d == 1:
            S.op("act", lambda e: e.activation(out=yst[:], in_=yacc[:], func=AF.Gelu), [yacc], [yst])
            S.dma(C["yact"][cc, :, :], yst[:], [yst], [], yst, queue="act")

    tables(iters[0])
    if len(iters) > 1:
        tables(iters[1])
    hd = head(iters[0])
    for i, it in enumerate(iters):
        if i + 2 < len(iters):
            tables(iters[i + 2])
        nhd = head(iters[i + 1]) if i + 1 < len(iters) else None
        tail(it, hd)
        hd = nhd


def phase_C2(S, C, T):
    A = S.alloc
    psF = C["psF"]
    YACT = A([128, 8, T], BF16, "YACT")
    for cc in range(8):
        S.dma(YACT[:, cc, :], C["yact"][cc, :, :], [], [YACT], YACT)
    war = Ring([A([128, 8, 128], BF16, f"wa{i}") for i in range(2)])
    wbr = Ring([A([128, 8, 128], BF16, f"wb{i}") for i in range(2)])
    sbr = Ring([A([128, 512], F32, f"sb{i}") for i in range(2)])
    gbr = Ring([A([128, 512], F32, f"gbt{i}") for i in range(2)])
    obr = Ring([A([128, 512], F32, f"mbo{i}") for i in range(3)])
    par = Ring(psF[0:3]); pbr = Ring(psF[3:6])
    wa_v = C["glu_a"].t.rearrange("(kc p) f -> p kc f", p=128)
    wb_v = C["glu_b"].t.rearrange("(kc p) f -> p kc f", p=128)
    for dc in range(16):
        wa = war.next(); wb = wbr.next()
        S.dma(wa[:], C["glu_a_bf"][dc], [], [wa], wa)
        S.dma(wb[:], C["glu_b_bf"][dc], [], [wb], wb)
        for p in range(T // 512):
            c0 = p * 512
            pa = par.next(); pb = pbr.next(); sb = sbr.next(); gbt = gbr.next(); ob = obr.next()
            S.dma(gbt[:], C["gbT"][dc, :, c0:c0 + 512], [], [gbt], gbt)
            for kc in range(8):
                S.op("pe", lambda e, pa=pa, wa=wa, kc=kc, c0=c0: e.matmul(pa[:], lhsT=wa[:, kc, :], rhs=YACT[:, kc, c0:c0 + 512], start=(kc == 0), stop=(kc == 7)), [wa, YACT], [pa])
            for kc in range(8):
                S.op("pe", lambda e, pb=pb, wb=wb, kc=kc, c0=c0: e.matmul(pb[:], lhsT=wb[:, kc, :], rhs=YACT[:, kc, c0:c0 + 512], start=(kc == 0), stop=(kc == 7)), [wb, YACT], [pb])
            S.op("act", lambda e, pb=pb, sb=sb: e.activation(out=sb[:], in_=pb[:], func=AF.Sigmoid), [pb], [sb])
            S.op("dve", lambda e, pa=pa, sb=sb: e.tensor_tensor(out=sb[:], in0=pa[:], in1=sb[:], op=ALU.mult), [pa, sb], [sb])
            S.op("pool", lambda e, sb=sb, gbt=gbt, ob=ob: e.tensor_tensor(out=ob[:], in0=sb[:], in1=gbt[:], op=ALU.mult), [sb, gbt], [ob])
            S.dma(C["MB"][dc, :, c0:c0 + 512], ob[:], [ob], [], ob, queue="act")


D = 2048
EPS = 1e-6


def v4(ap):
    return ap.rearrange("p (a b) -> p a b", b=128)


def phase_B(S, C, NSEQ, SEQ):
    A = S.alloc
    T = NSEQ * SEQ
    NBS = SEQ // 512
    NKB = SEQ // 128
    psF, psT, ident = C["psF"], C["psT"], C["ident_bf"]
    bias = A([128, 3, 16, 128], F32, "bias")
    S.dma(bias[:], C["alibi"][:].rearrange("p (j h t) -> p j h t", j=3, h=16), [], [bias], bias)
    esink = A([128, 16], F32, "esink")
    S.dma(esink[:], C["sink_b"][:], [], [esink], esink)
    S.op("act", lambda e: e.activation(out=esink[:], in_=esink[:], func=AF.Exp), [esink], [esink])
    ones_bf = A([128, 128], BF16, "ones_bf")
    S.op("dve", lambda e: e.memset(ones_bf[:], 1.0), [], [ones_bf])
    g2 = A([128, D], F32, "g2")
    S.dma(g2[:], C["ffn_g"][:], [], [g2], g2)
    epsD = A([128, 1], F32, "epsB")
    S.op("dve", lambda e: e.memset(epsD[:], EPS), [], [epsD])
    junk = A([128, D], BF16, "junkB")
    kring = Ring([A([128, 4, 768], BF16, f"kTb{i}") for i in range(2)])
    vring = Ring([A([128, 6, 512], BF16, f"vt{i}") for i in range(2)])
    qring = Ring([A([128, 4, 512], BF16, f"qTb{i}") for i in range(2)])
    etring = Ring([A([128, 3, 512], BF16, f"ET{i}") for i in range(2)])
    sbring = Ring([A([128, 512], F32, f"sbB{i}") for i in range(3)])
    dnring = Ring([A([128, 512], F32, f"dn{i}") for i in range(2)])
    OT = A([128, 16, 512], BF16, "OT")
    MT = A([128, 16, 512], BF16, "MT")
    woring = Ring([A([128, 16, 128], BF16, f"wo{i}") for i in range(2)])
    garing = Ring([A([128, 512], F32, f"gat{i}") for i in range(2)])
    mbring = Ring([A([128, 512], F32, f"mbt{i}") for i in range(2)])
    tmring = Ring([A([128, 512], F32, f"tm{i}") for i in range(2)])
    wtring = Ring([A([128, 16, 256], BF16, f"wout{i}") for i in range(2)])
    x1t = [A([128, D], F32, f"x1t{i}") for i in range(4)]
    h2ring = Ring([A([128, D], BF16, f"h2_{i}") for i in range(2)])
    h2Tb = OT
    ssring = Ring([A([128, 1], F32, f"ssB{i}") for i in range(4)])
    sring = Ring(psF[0:3])
    psO, psD = psF[3], psF[4]
    pring = Ring(psF[5:7])
    wo_v = C["w_o"].t.rearrange("(kc p) f -> p kc f", p=128)
    wout_v = C["w_out"].t.rearrange("(kc p) f -> p kc f", p=128)
    for tb in range(T // 512):
        b = tb // NBS; tbl = tb % NBS; c0 = tb * 512
        kTb = kring.next(); vt = vring.next()
        jbv = [0 <= 4 * tbl - 1 + jb < NKB for jb in range(6)]
        lo = jbv.index(True); hi = 6 - jbv[::-1].index(True)
        g0 = b * SEQ + (4 * tbl - 1 + lo) * 128; n = (hi - lo) * 128
        S.dma(kTb[:, :, lo * 128:hi * 128], C["kT"][:, :, g0:g0 + n].rearrange("k p t -> p k t"), [], [kTb], kTb)
        S.dma(vt[:, lo:hi, :], C["v"][g0:g0 + n, :].rearrange("(j p) f -> p j f", p=128), [], [vt], vt)
        for tl in range(4):
            r0 = c0 + tl * 128
            S.dma(x1t[tl][:], C["x"][r0:r0 + 128, :], [], [x1t[tl]], x1t[tl])
        qtbs = {}

        def stageS(kv, qb):
            if qb == 0:
                qTb = qring.next()
                S.dma(qTb[:], C["qT"][kv * 4:(kv + 1) * 4, :, c0:c0 + 512].rearrange("h p t -> p h t"), [], [qTb], qTb)
                qtbs[kv] = qTb
            qTb = qtbs[kv]
            js = [j for j in range(3) if jbv[qb + j]]
            ET = etring.next()
            for j in js:
                ps = sring.next(); sb = sbring.next()
                S.op("pe", lambda e, ps=ps, kTb=kTb, qTb=qTb, kv=kv, qb=qb, j=j: e.matmul(v4(ps[:]), lhsT=kTb[:, kv, (qb + j) * 128:(qb + j + 1) * 128], rhs=qTb[:, :, qb * 128:(qb + 1) * 128], start=True, stop=True), [kTb, qTb], [ps])
                S.op("dve", lambda e, ps=ps, sb=sb, j=j, kv=kv: e.tensor_tensor(out=v4(sb[:]), in0=v4(ps[:]), in1=bias[:, j, kv * 4:(kv + 1) * 4, :], op=ALU.add), [ps, bias], [sb])
                S.op("act", lambda e, sb=sb, ET=ET, j=j: e.activation(out=ET[:, j, :], in_=sb[:], func=AF.Exp), [sb], [ET])
            return (js, ET)

        def stagePV(kv, qb, st):
            js, ET = st
            for idx, j in enumerate(js):
                S.op("pe", lambda e, vt=vt, ET=ET, kv=kv, qb=qb, j=j, idx=idx, n=len(js): e.matmul(psO[:], lhsT=vt[:, qb + j, kv * 128:(kv + 1) * 128], rhs=ET[:, j, :], start=(idx == 0), stop=(idx == n - 1)), [vt, ET], [psO])
            for idx, j in enumerate(js):
                S.op("pe", lambda e, ET=ET, j=j, idx=idx, n=len(js): e.matmul(psD[:], lhsT=ones_bf[:], rhs=ET[:, j, :], start=(idx == 0), stop=(idx == n - 1)), [ones_bf, ET], [psD])
            dn = dnring.next()
            S.op("dve", lambda e, dn=dn, kv=kv: e.tensor_tensor(out=v4(dn[:]), in0=v4(psD[:]), in1=esink[:, kv * 4:(kv + 1) * 4].unsqueeze(2).to_broadcast([128, 4, 128]), op=ALU.add), [psD, esink], [dn])
            S.op("dve", lambda e, dn=dn: e.reciprocal(out=dn[:], in_=dn[:]), [dn], [dn])
            S.op("dve", lambda e, dn=dn, kv=kv, qb=qb: e.tensor_tensor(out=OT[:, kv * 4:(kv + 1) * 4, qb * 128:(qb + 1) * 128], in0=v4(psO[:]), in1=v4(dn[:]), op=ALU.mult), [psO, dn], [OT])

        groups = [(kv, qb) for kv in range(4) for qb in range(4)]
        stg = stageS(*groups[0])
        for gi_, g_ in enumerate(groups):
            nstg = stageS(*groups[gi_ + 1]) if gi_ + 1 < len(groups) else None
            stagePV(g_[0], g_[1], stg)
            stg = nstg
        for dc in range(16):
            wo = woring.next(); gat = garing.next(); mbt = mbring.next(); tm = tmring.next(); pa = pring.next()
            S.dma(wo[:], C["w_o_bf"][dc], [], [wo], wo)
            S.dma(gat[:], C["gaT"][dc, :, c0:c0 + 512], [], [gat], gat)
            S.dma(mbt[:], C["MB"][dc, :, c0:c0 + 512], [], [mbt], mbt)
            for h in range(16):
                S.op("pe", lambda e, pa=pa, wo=wo, h=h: e.matmul(pa[:], lhsT=wo[:, h, :], rhs=OT[:, h, :], start=(h == 0), stop=(h == 15)), [wo, OT], [pa])
            S.op("dve", lambda e, pa=pa, gat=gat, tm=tm: e.tensor_tensor(out=tm[:], in0=pa[:], in1=gat[:], op=ALU.mult), [pa, gat], [tm])
            S.op("pool", lambda e, tm=tm, mbt=mbt, dc=dc: e.tensor_tensor(out=MT[:, dc, :], in0=tm[:], in1=mbt[:], op=ALU.add), [tm, mbt], [MT])
        for cg in range(8):
            wt = wtring.next()
            S.dma(wt[:], C["w_out_bf"][cg], [], [wt], wt)
            for tl in range(4):
                px = pring.next()
                for kc in range(16):
                    S.op("pe", lambda e, px=px, wt=wt, kc=kc, tl=tl: e.matmul(px[:, 0:256], lhsT=MT[:, kc, tl * 128:(tl + 1) * 128], rhs=wt[:, kc, :], start=(kc == 0), stop=(kc == 15)), [MT, wt], [px])
                S.op("dve", lambda e, px=px, tl=tl, cg=cg: e.tensor_tensor(out=x1t[tl][:, cg * 256:(cg + 1) * 256], in0=px[:, 0:256], in1=x1t[tl][:, cg * 256:(cg + 1) * 256], op=ALU.add), [px, x1t[tl]], [x1t[tl]])
        for tl in range(4):
            r0 = c0 + tl * 128
            xt = x1t[tl]; ss = ssring.next(); h2 = h2ring.next()
            S.dma(C["x1"][r0:r0 + 128, :], xt[:], [xt], [], xt, queue="act")
            S.op("act", lambda e, xt=xt, ss=ss: e.activation(out=junk[:], in_=xt[:], func=AF.Square, accum_out=ss[:]), [xt], [junk, ss])
            S.op("act", lambda e, ss=ss: e.activation(out=ss[:], in_=ss[:], func=AF.Sqrt, scale=1.0 / D, bias=epsD[:]), [ss, epsD], [ss])
            S.op("dve", lambda e, ss=ss: e.reciprocal(out=ss[:], in_=ss[:]), [ss], [ss])
            S.op("dve", lambda e, xt=xt, ss=ss, h2=h2: e.scalar_tensor_tensor(out=h2[:], in0=xt[:], scalar=ss[:, 0:1], in1=g2[:], op0=ALU.mult, op1=ALU.mult), [xt, ss, g2], [h2])
            for half in range(2):
                for j in range(8):
                    kc = half * 8 + j
                    S.op("pe", lambda e, h2=h2, kc=kc, j=j: e.transpose(out=psT[:, j * 128:(j + 1) * 128], in_=h2[:, kc * 128:(kc + 1) * 128], identity=ident[:]), [h2, ident], [psT])
                S.op("act", lambda e, half=half, tl=tl: e.activation(out=h2Tb[:, half * 8:half * 8 + 8, tl * 128:(tl + 1) * 128], in_=v4(psT[:]), func=AF.Copy), [psT], [h2Tb])
        S.dma(C["h2T"][:, :, c0:c0 + 512], h2Tb[:], [h2Tb], [], h2Tb, queue="act")


D = 2048
NEG = -1.0e30
MARGIN = 1.0e-4


def phase_P0(S, C):
    A = S.alloc
    fr = Ring([A([128, 4096], F32, f"cvf{i}") for i in range(3)])
    br = Ring([A([128, 4096], BF16, f"cvb{i}") for i in range(3)])
    engs = ["act", "dve", "pool"]
    kk = [0]

    def cast(f, b, cw):
        en = engs[kk[0] % 3]; kk[0] += 1
        if en == "act":
            S.op("act", lambda e, f=f, b=b, cw=cw: e.activation(out=b[:, 0:cw], in_=f[:, 0:cw], func=AF.Copy), [f], [b])
        else:
            S.op(en, lambda e, f=f, b=b, cw=cw: e.tensor_copy(out=b[:, 0:cw], in_=f[:, 0:cw]), [f], [b])
    for src, dst, K, F, fw in ((C["w_in"], C["w_in_bf"], 2048, 8192, 256), (C["w_o"], C["w_o_bf"], 2048, 2048, 128),
                               (C["w_out"], C["w_out_bf"], 2048, 2048, 256), (C["glu_a"], C["glu_a_bf"], 1024, 2048, 128),
                               (C["glu_b"], C["glu_b_bf"], 1024, 2048, 128), (C["peer_wq"], C["wq_bf"], 2048, 2048, 128)):
        cw = min(F, 4096)
        for kc in range(K // 128):
            for c in range(F // cw):
                f = fr.next(); b = br.next()
                S.dma(f[:, 0:cw], src[kc * 128:(kc + 1) * 128, c * cw:(c + 1) * cw], [], [f], f)
                cast(f, b, cw)
                ng = cw // fw
                S.dma(dst[c * ng:(c + 1) * ng, :, kc, :].rearrange("g p f -> p g f"), b[:, 0:cw].rearrange("p (g f) -> p g f", f=fw), [b], [], b, queue="act")


def peer_conv_gen(S, C):
    A = S.alloc
    fr = Ring([A([128, 4096], F32, f"pcf{i}") for i in range(3)])
    br = Ring([A([128, 4096], BF16, f"pcb{i}") for i in range(3)])
    for src, dst, R, Cc in ((C["peer_downT"], C["downT_bf"], 2048, 16384), (C["peer_up"], C["up_bf"], 16384, 2048)):
        cw = min(Cc, 4096)
        rr = 4096 // cw
        for r in range(0, R // 128, rr):
            for c in range(Cc // cw):
                f = fr.next(); b = br.next()
                for q in range(rr):
                    S.dma(f[:, q * cw:(q + 1) * cw], src[(r + q) * 128:(r + q + 1) * 128, c * cw:(c + 1) * cw], [], [f], f)
                S.op("pool", lambda e, f=f, b=b: e.tensor_copy(out=b[:], in_=f[:]), [f], [b])
                for q in range(rr):
                    S.dma(dst[(r + q) * 128:(r + q + 1) * 128, c * cw:(c + 1) * cw], b[:, q * cw:(q + 1) * cw], [b], [], b, queue="act")
                yield


def phase_D(S, C, T):
    A = S.alloc
    psS, psBig, psT, ident = C["psS"], C["psBig"], C["psT"], C["ident_bf"]
    subk = A([128, 16, 128], F32, "subk")
    S.dma(subk[:], C["subk"][:], [], [subk], subk)
    h2Tb = A([128, 16, 512], BF16, "h2TbD")
    SC = A([128, 4, 16, 128], F32, "SC")
    SCv = SC[:].rearrange("p t (h s) k -> p t h s k", s=2)
    SCt = [Buf(SC[:, tl, :, :], f"SCt{tl}") for tl in range(4)]
    V16t = []; T16t = []; smalls = []
    for tl in range(4):
        Vb_ = A([128, 16, 16], F32, f"V16_{tl}")
        V16t.append({"t": Vb_, "v": Vb_[:].rearrange("p (h s) k -> p h s k", s=2),
                     "a": [Buf(Vb_[:, sg, 0:8]) for sg in range(16)], "b": [Buf(Vb_[:, sg, 8:16]) for sg in range(16)]})
        Tb_ = A([128, 8, 16], F32, f"T16_{tl}")
        T16t.append({"t": Tb_, "a": [Buf(Tb_[:, h, 0:8]) for h in range(8)], "b": [Buf(Tb_[:, h, 8:16]) for h in range(8)]})
        OFF_ = A([128, 16], F32, f"OFF{tl}")
        smalls.append({"OFF": OFF_, "OFFv": OFF_[:].rearrange("p (h s) -> p h s", s=2), "EX": A([128, 8, 16], F32, f"EX{tl}"),
                       "Z": A([128, 8], F32, f"Z{tl}"), "thrE": A([128, 8], F32, f"thrE{tl}")})
    tms = [A([128, 128], F32, f"tmk{i}") for i in range(8)] * 2
    cand = A([128, 8, 256], F32, "cand")
    cand4 = cand[:].rearrange("p h (a b) -> p h a b", b=16)
    tcs = [A([128, 256], F32, f"tmc{i}") for i in range(4)] * 2
    DG = [A([128, 8, 128], BF16, f"DG{i}") for i in range(4)]
    dTr = Ring([A([128, 16, 256], BF16, f"dT{i}") for i in range(2)])
    upr = Ring([A([128, 2, 2048], BF16, f"upc{i}") for i in range(2)])
    GLs = [A([128, 4, 256], BF16, f"GL{i}") for i in range(2)]
    Eps = [A([128, 8, 2, 128], F32, f"Ep{i}") for i in range(2)]
    Mp0 = A([128, 8, 256], BF16, "Mp0")
    Rp0 = A([128, 8, 256], BF16, "Rp0")
    Mk0 = A([128, 8, 256], BF16, "Mk0")
    neg1 = A([128, 1], F32, "neg1")
    S.op("dve", lambda e: e.memset(neg1[:], -1.0), [], [neg1])
    Wtr = Ring([A([128, 256], BF16, f"Wt{i}") for i in range(2)])
    WTr = Ring([A([128, 256], BF16, f"WT{i}") for i in range(2)])
    oacc = [A([128, D], F32, f"oacc{i}") for i in range(4)]
    oaccH = [[Buf(oacc[i][:, h * 1024:(h + 1) * 1024], f"oaccH{i}{h}") for h in range(2)] for i in range(4)]
    psBh = [Buf(psBig[:, h * 1024:(h + 1) * 1024], f"psBh{h}") for h in range(2)]
    wqr = Ring([A([128, 16, 128], BF16, f"wq{i}") for i in range(2)])
    qcr = Ring([A([128, 512], F32, f"qc{i}") for i in range(2)])
    par = Ring(psS[0:2])
    pg = psS[2]
    wq_v = C["peer_wq"].t.rearrange("(kc p) f -> p kc f", p=128)
    dT_v = C["downT_bf"].t.rearrange("(kc p) e -> p kc e", p=128)
    for tb in range(T // 512):
        c0 = tb * 512
        S.dma(h2Tb[:], C["h2T"][:, :, c0:c0 + 512], [], [h2Tb], h2Tb)
        for tl in range(4):
            S.dma(oacc[tl][:], C["x1"][c0 + tl * 128:c0 + (tl + 1) * 128, :], [], [oacc[tl]] + oaccH[tl], oacc[tl])
        for fch in range(16):
            wq = wqr.next(); qc = qcr.next(); pq = par.next()
            S.dma(wq[:], C["wq_bf"][fch], [], [wq], wq)
            for kc in range(16):
                S.op("pe", lambda e, pq=pq, wq=wq, kc=kc: e.matmul(pq[:], lhsT=wq[:, kc, :], rhs=h2Tb[:, kc, :], start=(kc == 0), stop=(kc == 15)), [wq, h2Tb], [pq])
            S.op("act", lambda e, pq=pq, qc=qc: e.activation(out=qc[:], in_=pq[:], func=AF.Copy), [pq], [qc])
            for tl in range(4):
                S.op("pe", lambda e, qc=qc, tl=tl, fch=fch: e.matmul(pg[:, tl * 128:(tl + 1) * 128], lhsT=qc[:, tl * 128:(tl + 1) * 128], rhs=subk[:, fch, :], start=True, stop=True), [qc, subk], [pg])
            S.op("act", lambda e, fch=fch: e.activation(out=SC[:, :, fch, :], in_=pg[:].rearrange("p (a b) -> p a b", b=128), func=AF.Copy), [pg], SCt)
        def batches(tl):
            V = V16t[tl]; Tt = T16t[tl]
            for sh in range(2):
                segs = range(sh * 8, sh * 8 + 8)
                for seg in segs:
                    S.op("dve", lambda e, tl=tl, seg=seg, V=V: e.max(out=V["a"][seg][:], in_=SC[:, tl, seg, :]), [SCt[tl]], [V["a"][seg]])
                for seg in segs:
                    S.op("dve", lambda e, tl=tl, seg=seg, V=V: e.match_replace(out=tms[seg][:], in_to_replace=V["a"][seg][:], in_values=SC[:, tl, seg, :], imm_value=NEG), [SCt[tl], V["a"][seg]], [tms[seg]])
                for seg in segs:
                    S.op("dve", lambda e, seg=seg, V=V: e.max(out=V["b"][seg][:], in_=tms[seg][:]), [tms[seg]], [V["b"][seg]])
            Vv = V["v"]
            S.op("dve", lambda e, Vv=Vv: e.tensor_tensor(out=cand4, in0=Vv[:, :, 0, :].unsqueeze(3).to_broadcast([128, 8, 16, 16]), in1=Vv[:, :, 1, :].unsqueeze(2).to_broadcast([128, 8, 16, 16]), op=ALU.add), V["a"] + V["b"], [cand])
            for hh in range(2):
                hs_ = range(hh * 4, hh * 4 + 4)
                for h in hs_:
                    S.op("dve", lambda e, h=h, Tt=Tt: e.max(out=Tt["a"][h][:], in_=cand[:, h, :]), [cand], [Tt["a"][h]])
                for h in hs_:
                    S.op("dve", lambda e, h=h, Tt=Tt: e.match_replace(out=tcs[h][:], in_to_replace=Tt["a"][h][:], in_values=cand[:, h, :], imm_value=NEG), [cand, Tt["a"][h]], [tcs[h]])
                for h in hs_:
                    S.op("dve", lambda e, h=h, Tt=Tt: e.max(out=Tt["b"][h][:], in_=tcs[h][:]), [tcs[h]], [Tt["b"][h]])

        def tail(tl):
            V = V16t[tl]; Tt = T16t[tl]; Vv = V["v"]; T16 = Tt["t"]; sm = smalls[tl]
            OFF, OFFv, EX, Z, thrE = sm["OFF"], sm["OFFv"], sm["EX"], sm["Z"], sm["thrE"]
            rdV = V["a"] + V["b"]; rdT = Tt["a"] + Tt["b"]
            S.op("dve", lambda e: e.tensor_copy(out=OFFv[:, :, 0], in_=Vv[:, :, 0, 0]), rdV, [OFF])
            S.op("dve", lambda e: e.scalar_tensor_tensor(out=OFFv[:, :, 1], in0=T16[:, :, 15], scalar=-MARGIN, in1=Vv[:, :, 0, 0], op0=ALU.add, op1=ALU.subtract), rdT + rdV, [OFF])
            S.op("dve", lambda e: e.tensor_tensor(out=EX[:], in0=T16[:], in1=T16[:, :, 0:1].to_broadcast([128, 8, 16]), op=ALU.subtract), rdT, [EX])
            S.op("act", lambda e: e.activation(out=EX[:], in_=EX[:], func=AF.Exp), [EX], [EX])
            S.op("dve", lambda e: e.tensor_reduce(out=Z[:], in_=EX[:], axis=AX.X, op=ALU.add), [EX], [Z])
            S.op("dve", lambda e: e.reciprocal(out=Z[:], in_=Z[:]), [Z], [Z])
            S.op("dve", lambda e: e.scalar_tensor_tensor(out=thrE[:], in0=EX[:, :, 15], scalar=float(math.exp(-MARGIN)), in1=Z[:], op0=ALU.mult, op1=ALU.mult), [EX, Z], [thrE])
            S.op("dve", lambda e, tl=tl: e.tensor_tensor(out=DG[tl][:], in0=ident[:].unsqueeze(1).to_broadcast([128, 8, 128]), in1=thrE[:].unsqueeze(2).to_broadcast([128, 8, 128]), op=ALU.mult), [ident, thrE], [DG[tl]])
            S.op("dve", lambda e, tl=tl: e.tensor_tensor(out=SC[:, tl, :, :], in0=SC[:, tl, :, :], in1=OFF[:].unsqueeze(2).to_broadcast([128, 16, 128]), op=ALU.subtract), [SCt[tl], OFF], [SCt[tl]])
            S.op("act", lambda e, tl=tl: e.activation(out=SC[:, tl, :, :], in_=SC[:, tl, :, :], func=AF.Exp), [SCt[tl]], [SCt[tl]])

        batches(0)
        for tl in range(1, 4):
            batches(tl)
            tail(tl - 1)
        tail(3)
        NGRP = 64
        dts = {}; ups = {}; cur = {}

        def load_w(eg):
            dT = dTr.next(); upc = upr.next()
            S.dma(dT[:], dT_v[:, :, eg * 256:(eg + 1) * 256], [], [dT], dT)
            S.dma(upc[:], C["up_bf"][eg * 256:(eg + 1) * 256, :].rearrange("(ec p) d -> p ec d", p=128), [], [upc], upc)
            dts[eg] = dT; ups[eg] = upc

        def stage1(eg, tl, part=None):
            dT = dts[eg]; GL = GLs[eg % 2]
            if part in (None, 0):
                cur["pa"] = par.next()
            pa = cur["pa"]
            kcs = range(16) if part is None else range(part * 8, part * 8 + 8)
            for kc in kcs:
                S.op("pe", lambda e, pa=pa, kc=kc, tl=tl, dT=dT: e.matmul(pa[:, 0:256], lhsT=h2Tb[:, kc, tl * 128:(tl + 1) * 128], rhs=dT[:, kc, :], start=(kc == 0), stop=(kc == 15)), [h2Tb, dT], [pa])
            if part in (None, 1):
                S.op("act", lambda e, pa=pa, tl=tl, GL=GL: e.activation(out=GL[:, tl, :], in_=pa[:, 0:256], func=AF.Gelu), [pa], [GL])

        def emask_a(k):
            eg, tl = divmod(k, 4)
            Ep = Eps[k % 2]
            S.op("pool", lambda e, tl=tl, eg=eg, Ep=Ep: e.tensor_tensor(out=Ep[:], in0=SCv[:, tl, :, 0, eg * 2:(eg + 1) * 2].unsqueeze(3).to_broadcast([128, 8, 2, 128]), in1=SCv[:, tl, :, 1, :].unsqueeze(2).to_broadcast([128, 8, 2, 128]), op=ALU.mult), [SCt[tl]], [Ep])
            if k % 2 == 1:
                S.op("act", lambda e, Ep=Ep: e.activation(out=Rp0[:].rearrange("p h e -> p (h e)"), in_=Ep[:].rearrange("p h a b -> p (h a b)"), func=AF.Relu, bias=neg1[:]), [Ep, neg1], [Rp0])

        def emask_b(k):
            Ep = Eps[k % 2]
            if k % 2 == 0:
                S.op("dve", lambda e, Ep=Ep: e.scalar_tensor_tensor(out=Mp0[:].rearrange("p h e -> p (h e)"), in0=Ep[:].rearrange("p h a b -> p (h a b)"), scalar=1.0, in1=Ep[:].rearrange("p h a b -> p (h a b)"), op0=ALU.is_ge, op1=ALU.mult), [Ep], [Mp0])

        def emask_c(k):
            if k % 2 == 1:
                S.op("act", lambda e: e.activation(out=Mk0[:], in_=Rp0[:], func=AF.Sign), [Rp0], [Mk0])

        load_w(0)
        for tl in range(4):
            stage1(0, tl)
        emask_a(0); emask_b(0); emask_c(0)
        for k in range(NGRP * 4):
            eg, tl = divmod(k, 4)
            if tl == 0 and eg + 1 < NGRP:
                load_w(eg + 1)
            GL = GLs[eg % 2]; upc = ups[eg]
            Wt = Wtr.next(); WT = WTr.next()
            more = k + 1 < NGRP * 4
            if more:
                emask_a(k + 1)
            if k % 2 == 0:
                for h in range(8):
                    S.op("pe", lambda e, tl=tl, h=h: e.matmul(pg[:, 0:256], lhsT=DG[tl][:, h, :], rhs=Mp0[:, h, :], start=(h == 0), stop=(h == 7)), [DG[tl], Mp0], [pg])
            else:
                for h in range(8):
                    S.op("pe", lambda e, tl=tl, h=h: e.matmul(pg[:, 0:256], lhsT=DG[tl][:, h, :], rhs=Rp0[:, h, :], start=(h == 0), stop=False), [DG[tl], Rp0], [pg])
                for h in range(8):
                    S.op("pe", lambda e, tl=tl, h=h: e.matmul(pg[:, 0:256], lhsT=DG[tl][:, h, :], rhs=Mk0[:, h, :], start=False, stop=(h == 7)), [DG[tl], Mk0], [pg])
            S.op("dve", lambda e, tl=tl, Wt=Wt, GL=GL: e.tensor_tensor(out=Wt[:], in0=pg[:, 0:256], in1=GL[:, tl, :], op=ALU.mult), [pg, GL], [Wt])
            if more:
                emask_b(k + 1)
            if eg + 1 < NGRP:
                stage1(eg + 1, tl, 0)
            for ec in range(2):
                S.op("pe", lambda e, Wt=Wt, ec=ec: e.transpose(out=psT[:, ec * 128:(ec + 1) * 128], in_=Wt[:, ec * 128:(ec + 1) * 128], identity=ident[:]), [Wt, ident], [psT])
            S.op("act", lambda e, WT=WT: e.activation(out=WT[:], in_=psT[:, 0:256], func=AF.Copy), [psT], [WT])
            if more:
                emask_c(k + 1)
            if eg + 1 < NGRP:
                stage1(eg + 1, tl, 1)
            if "dbg" in C and k == C["dbg"]["k"]:
                dd = C["dbg"]
                S.dma(dd["Ep"][:], Eps[k % 2][:].rearrange("p h a b -> p (h a b)"), [Eps[k % 2]], [], Eps[k % 2], queue="act")
                S.dma(dd["Mp"][:], Mp0[:].rearrange("p h e -> p (h e)"), [Mp0], [], Mp0, queue="act")
                S.dma(dd["Wt"][:], Wt[:], [Wt], [], Wt, queue="act")
                S.dma(dd["WT"][:], WT[:], [WT], [], WT, queue="act")
                S.dma(dd["GL"][:], GL[:].rearrange("p a b -> p (a b)"), [GL], [], GL, queue="act")
                S.dma(dd["SC"][:], SC[:].rearrange("p a b c -> p (a b c)"), SCt, [], SC, queue="act")
                S.dma(dd["DG"][:], DG[tl][:].rearrange("p a b -> p (a b)"), [DG[tl]], [], DG[tl], queue="act")
            for half in range(2):
                pbh = psBh[half]
                for dgp in range(2 * half, 2 * half + 2):
                    for ec in range(2):
                        S.op("pe", lambda e, WT=WT, dgp=dgp, ec=ec, upc=upc, pbh=pbh: e.matmul(pbh[:, (dgp % 2) * 512:(dgp % 2 + 1) * 512], lhsT=WT[:, ec * 128:(ec + 1) * 128], rhs=upc[:, ec, dgp * 512:(dgp + 1) * 512], start=(ec == 0), stop=(ec == 1)), [WT, upc], [pbh])
                S.op("dve", lambda e, tl=tl, half=half, pbh=pbh: e.tensor_tensor(out=oaccH[tl][half][:], in0=pbh[:], in1=oaccH[tl][half][:], op=ALU.add), [pbh, oaccH[tl][half]], [oaccH[tl][half]])
        for tl in range(4):
            S.dma(C["out"][c0 + tl * 128:c0 + (tl + 1) * 128, :], oacc[tl][:], [oacc[tl]] + oaccH[tl], [], oacc[tl], queue="act")


def ssm_host(inp):
    f = np.float32
    a_re = inp["ssm_a_re"][0]; a_im = inp["ssm_a_im"][0]; ls = inp["ssm_log_step"][0]
    def lay1(a):
        t = a.reshape(128, 64).T
        return np.ascontiguousarray(np.concatenate([t, t], 0), dtype=f)
    o = {}
    o["s_aim"] = lay1(a_im); o["s_are"] = lay1(a_re)
    o["s_ls"] = np.ascontiguousarray(np.broadcast_to(ls.reshape(1, 128), (128, 128)), dtype=f)
    bre_t = inp["ssm_b_re"][0].reshape(128, 64, 16).transpose(1, 0, 2)
    bim_t = inp["ssm_b_im"][0].reshape(128, 64, 16).transpose(1, 0, 2)
    o["s_x1"] = np.ascontiguousarray(np.concatenate([bre_t, bim_t], 0), dtype=f)
    o["s_x2"] = np.ascontiguousarray(np.concatenate([bim_t, bre_t], 0), dtype=f)
    cre_t = inp["ssm_c_re"][0].reshape(128, 16, 64).transpose(2, 0, 1)
    cim_t = inp["ssm_c_im"][0].reshape(128, 16, 64).transpose(2, 0, 1)
    o["s_ca"] = np.ascontiguousarray(np.concatenate([cre_t, cim_t], 0), dtype=f)
    o["s_cb"] = np.ascontiguousarray(np.concatenate([cim_t, cre_t], 0), dtype=f)
    sg = np.zeros((128, 2), f); sg[:64, 0] = -1; sg[64:, 0] = 1; sg[:64, 1] = 1; sg[64:, 1] = -1
    o["s_sgn"] = sg
    o["ssm_d"] = np.ascontiguousarray(inp["ssm_d"][0].reshape(8, 128).T, dtype=f)
    o["iota"] = np.ascontiguousarray(np.broadcast_to(np.arange(2048, dtype=f), (128, 2048)))
    return o


def attn_host(inp):
    f = np.float32
    slopes = (2.0 ** (-8.0 * (np.arange(16, dtype=np.float64) + 1) / 16))
    tk = np.arange(128)[:, None, None, None]; j = np.arange(3)[None, :, None, None]; tq = np.arange(128)[None, None, None, :]
    dist = np.abs(tq - tk - 128 * (j - 1)).astype(np.float64)
    b = -slopes[None, None, :, None] * dist
    b = np.where(dist <= 128, b, -30000.0)
    o = {"alibi": np.ascontiguousarray(b.reshape(128, 3 * 16 * 128), dtype=f)}
    o["sink_b"] = np.ascontiguousarray(np.broadcast_to(inp["attn_sink"][0][None, :], (128, 16)), dtype=f)
    o["ffn_g"] = np.ascontiguousarray(np.broadcast_to(inp["ffn_norm_g"][0][None, :], (128, 2048)), dtype=f)
    return o


def build_nc(NSEQ=2, SEQ=2048, dbg=False, dbgk=0):
    T = NSEQ * SEQ
    nc = bass.Bass("TRN2", target_bir_lowering=False)
    st = ExitStack()
    with st:
        S = Sched(nc, st)
        S.arena_init(200 * 1024)
        C = {"wq": "pool"}
        EI = "ExternalInput"
        C["x"] = S.dram("x", [T, D], F32, EI)
        for nm, sh in [("w_in", [D, 8192]), ("mix_g", [128, D]), ("qk_g", [128, 2]), ("ident", [128, 128]),
                       ("s_aim", [128, 128]), ("s_are", [128, 128]), ("s_ls", [128, 128]), ("s_x1", [128, 128, 16]), ("s_x2", [128, 128, 16]),
                       ("s_ca", [128, 128, 16]), ("s_cb", [128, 128, 16]), ("s_sgn", [128, 2]), ("ssm_d", [128, 8]), ("iota", [128, 2048]),
                       ("glu_a", [1024, 2048]), ("glu_b", [1024, 2048]), ("alibi", [128, 3 * 16 * 128]), ("sink_b", [128, 16]), ("ffn_g", [128, 2048]),
                       ("w_o", [2048, 2048]), ("w_out", [2048, 2048]), ("peer_wq", [2048, 2048]), ("subk", [128, 16, 128]),
                       ("peer_downT", [2048, 16384]), ("peer_up", [16384, 2048])]:
            C[nm] = S.dram(nm, sh, F32, EI)
        k = "Internal"
        C["qT"] = S.dram("qT", [16, 128, T], BF16, k); C["kT"] = S.dram("kT", [4, 128, T], BF16, k)
        C["v"] = S.dram("v", [T, 512], BF16, k); C["uT"] = S.dram("uT", [8, 128, T], BF16, k)
        C["gaT"] = S.dram("gaT", [16, 128, T], F32, k); C["gbT"] = S.dram("gbT", [16, 128, T], F32, k)
        C["prm"] = S.dram("prm", [128, 128, 64], BF16, k)
        C["MB"] = S.dram("MB", [16, 128, T], F32, k)
        C["yact"] = S.dram("yact", [8, 128, T], BF16, k)
        C["x1"] = S.dram("x1", [T, D], F32, k)
        C["h2T"] = S.dram("h2T", [128, 16, T], BF16, k)
        C["w_in_bf"] = S.dram("w_in_bf", [32, 128, 16, 256], BF16, k)
        C["w_o_bf"] = S.dram("w_o_bf", [16, 128, 16, 128], BF16, k)
        C["w_out_bf"] = S.dram("w_out_bf", [8, 128, 16, 256], BF16, k)
        C["glu_a_bf"] = S.dram("glu_a_bf", [16, 128, 8, 128], BF16, k)
        C["glu_b_bf"] = S.dram("glu_b_bf", [16, 128, 8, 128], BF16, k)
        C["wq_bf"] = S.dram("wq_bf", [16, 128, 16, 128], BF16, k)
        C["downT_bf"] = S.dram("downT_bf", [2048, 16384], BF16, k)
        C["up_bf"] = S.dram("up_bf", [16384, 2048], BF16, k)
        C["out"] = S.dram("out", [T, D], F32, "ExternalOutput")
        if dbg:
            EO = "ExternalOutput"
            C["dbg"] = {"k": dbgk, "Ep": S.dram("d_Ep", [128, 2048], F32, EO), "Mp": S.dram("d_Mp", [128, 2048], BF16, EO),
                        "Wt": S.dram("d_Wt", [128, 256], BF16, EO), "WT": S.dram("d_WT", [128, 256], BF16, EO),
                        "GL": S.dram("d_GL", [128, 1024], BF16, EO), "SC": S.dram("d_SC", [128, 8192], F32, EO),
                        "DG": S.dram("d_DG", [128, 1024], BF16, EO)}
        C["psBig"] = S.psum([128, 2048], F32, "psBig")
        C["psS"] = [S.psum([128, 512], F32, f"psS{i}") for i in range(3)]
        C["psF"] = C["psS"] + [Buf(C["psBig"].t[:, i * 512:(i + 1) * 512], f"psBv{i}") for i in range(4)]
        C["psT"] = S.psum([128, 1024], BF16, "psT")
        idf = S.sbuf([128, 128], F32, "idf"); C["ident_bf"] = S.sbuf([128, 128], BF16, "idb")
        C["ident_f"] = idf
        S.dma(idf[:], C["ident"][:], [], [idf], idf)
        S.op("dve", lambda e: e.tensor_copy(out=C["ident_bf"][:], in_=idf[:]), [idf], [C["ident_bf"]])
        phase_P0(S, C)
        S.arena_reset(); phase_A(S, C, T)
        S.arena_reset(); phase_C0(S, C)
        S.arena_reset(); phase_C1(S, C, NSEQ, SEQ)
        S.arena_reset(); phase_C2(S, C, T)
        S.arena_reset(); phase_B(S, C, NSEQ, SEQ)
        S.arena_reset(); phase_D(S, C, T)
        S.barrier()
        for e in S.ENG:
            S.op(e, lambda e: e.nop(), [], [])
        S.emit()
        build_nc.stats = (len(S.ops), S.nsem)
    return nc


def host_inputs(inp):
    f = np.float32
    c = lambda a: np.ascontiguousarray(a, dtype=f)
    o = {}
    o["w_in"] = c(inp["w_in"][0])
    o["mix_g"] = c(np.broadcast_to(inp["mix_norm_g"][0][None, :], (128, D)))
    o["qk_g"] = c(np.stack([inp["q_norm_g"][0], inp["k_norm_g"][0]], 1))
    o["ident"] = np.eye(128, dtype=f)
    o.update(ssm_host(inp))
    o.update(attn_host(inp))
    o["glu_a"] = c(inp["glu_w_a"][0]); o["glu_b"] = c(inp["glu_w_b"][0])
    o["w_o"] = c(inp["w_attn_o"][0]); o["w_out"] = c(inp["w_out"][0])
    o["peer_wq"] = c(inp["peer_w_query"][0])
    k1 = inp["peer_sub_keys_1"][0]; k2 = inp["peer_sub_keys_2"][0]
    sk = np.stack([k1, k2], 1)
    o["subk"] = c(sk.transpose(3, 0, 1, 2).reshape(128, 16, 128))
    o["peer_downT"] = c(inp["peer_down"][0].T)
    o["peer_up"] = c(inp["peer_up"][0])
    return o


_NC_CACHE = {}


def kernel(**inputs):
    x = np.asarray(inputs["x"], dtype=np.float32)
    B, SEQ, _ = x.shape
    ncores = 8
    NSEQ = B // ncores
    key = (NSEQ, SEQ)
    if key not in _NC_CACHE:
        _NC_CACHE[key] = build_nc(NSEQ, SEQ)
    nc = _NC_CACHE[key]
    shared = host_inputs(inputs)
    in_maps = []
    for i in range(ncores):
        m = dict(shared)
        m["x"] = np.ascontiguousarray(x[i * NSEQ:(i + 1) * NSEQ].reshape(NSEQ * SEQ, D))
        in_maps.append(m)
    res = run_bass_kernel_spmd(nc, in_maps, core_ids=list(range(ncores)))
    outs = [np.asarray(r["out"], dtype=np.float32).reshape(NSEQ, SEQ, D) for r in res.results]
    return np.concatenate(outs, axis=0)
```
